# Optimizing a Trainium2 kernel written in Bass

```python
import math
import jax
import jax.numpy as jnp
from jax import lax
import numpy as np

D_MODEL = 2048
BATCH = 2
SEQ = 4096
DEPTH = 4

GRID_W = 64
CTX_LEN = 256
EPS = 1e-6
N_MOD = 6

S5_WIDTH = 1024
S5_GROUP = 16
S5_GROUPS = S5_WIDTH // S5_GROUP
S5_STATE = 64

SSD_WIDTH = 1024
SSD_HEAD_DIM = 64
SSD_HEADS = SSD_WIDTH // SSD_HEAD_DIM
SSD_GROUPS = 2
SSD_STATE = 128
SSD_CONV = 5
SSD_CHUNK = 128
SSD_XBC = SSD_WIDTH + 2 * SSD_GROUPS * SSD_STATE

D_IN = S5_WIDTH + SSD_WIDTH + SSD_XBC + 2 * SSD_HEADS
D_MIX = S5_WIDTH + SSD_WIDTH

N_EXPERTS = 16
CAPACITY_FACTOR = 2
D_FF = 1536

kernel_name = 'hybrid_s5_ssd_ecmoe_diffusion_trunk'

F32 = jnp.float32


def rmsnorm(x, w):
    xf = x.astype(F32)
    y = xf * lax.rsqrt(jnp.mean(xf * xf, axis=-1, keepdims=True) + EPS)
    return (y * w.astype(F32)).astype(x.dtype)


def to_scan_order(x, col_major):
    if not col_major:
        return x
    b, n = x.shape[:2]
    rows = n // GRID_W
    return x.reshape((b, rows, GRID_W) + x.shape[2:]).swapaxes(1, 2).reshape(x.shape)


def from_scan_order(x, col_major):
    if not col_major:
        return x
    b, n = x.shape[:2]
    rows = n // GRID_W
    return x.reshape((b, GRID_W, rows) + x.shape[2:]).swapaxes(1, 2).reshape(x.shape)


def dir_seq(a, lc, reverse):
    if not reverse:
        return a
    return jnp.concatenate([jnp.flip(a[:, :lc], 1), jnp.flip(a[:, lc:], 1)], axis=1)


def dwconv_centred(x, w, b):
    pad = w.shape[0] // 2
    y = lax.conv_general_dilated(x, w[:, None, :].astype(x.dtype), window_strides=(1,),
                                 padding=((pad, pad),), dimension_numbers=('NWC', 'WIO', 'NWC'),
                                 feature_group_count=x.shape[-1])
    return y + b.astype(x.dtype)


def s5_discretise(lam_re, lam_im, log_step, b_re, b_im):
    step = jnp.exp(log_step)[:, None]
    mag = jnp.exp(lam_re * step)
    ang = lam_im * step
    lb_re, lb_im = mag * jnp.cos(ang), mag * jnp.sin(ang)
    den = lam_re * lam_re + lam_im * lam_im
    nr = lb_re - 1.0
    f_re = (nr * lam_re + lb_im * lam_im) / den
    f_im = (lb_im * lam_re - nr * lam_im) / den
    bb_re = f_re[..., None] * b_re - f_im[..., None] * b_im
    bb_im = f_re[..., None] * b_im + f_im[..., None] * b_re
    return lb_re, lb_im, bb_re, bb_im


def _complex_affine_combine(e1, e2):
    a1r, a1i, b1r, b1i = e1
    a2r, a2i, b2r, b2i = e2
    return (a2r * a1r - a2i * a1i, a2r * a1i + a2i * a1r,
            a2r * b1r - a2i * b1i + b2r, a2r * b1i + a2i * b1r + b2i)


def s5_states(u, lb_re, lb_im, bb_re, bb_im):
    bu_re = jnp.einsum('btgh,gph->btgp', u, bb_re)
    bu_im = jnp.einsum('btgh,gph->btgp', u, bb_im)
    a_re = jnp.broadcast_to(lb_re, bu_re.shape)
    a_im = jnp.broadcast_to(lb_im, bu_im.shape)
    _, _, h_re, h_im = lax.associative_scan(_complex_affine_combine, (a_re, a_im, bu_re, bu_im), axis=1)
    return h_re, h_im


def s5_mixer(u, lc, lam_re, lam_im, log_step, b_re, b_im, c_re, c_im, d_skip, w_glu, b_glu):
    bsz, t, _ = u.shape
    uf = u.astype(F32).reshape(bsz, t, S5_GROUPS, S5_GROUP)
    b_re, b_im = b_re.astype(F32), b_im.astype(F32)
    h_re, h_im = 0.0, 0.0
    for d in range(2):
        rev = d == 1
        lb_re, lb_im, bb_re, bb_im = s5_discretise(lam_re[d].astype(F32), lam_im[d].astype(F32),
                                                   log_step[d].astype(F32), b_re, b_im)
        s_re, s_im = s5_states(dir_seq(uf, lc, rev), lb_re, lb_im, bb_re, bb_im)
        h_re = h_re + dir_seq(s_re, lc, rev)
        h_im = h_im + dir_seq(s_im, lc, rev)
    y = (jnp.einsum('btgp,ghp->btgh', h_re, c_re.astype(F32))
         - jnp.einsum('btgp,ghp->btgh', h_im, c_im.astype(F32))
         + d_skip.astype(F32).reshape(S5_GROUPS, S5_GROUP) * uf)
    y = jax.nn.gelu(y.reshape(bsz, t, S5_WIDTH)).astype(u.dtype)
    gl = y @ w_glu + b_glu
    return gl[..., :S5_WIDTH] * jax.nn.sigmoid(gl[..., S5_WIDTH:])


def ssd_chunked(x, dt, a, bm, cm):
    b, t, h, p = x.shape
    g, n = bm.shape[2], bm.shape[3]
    j = h // g
    nc, l = t // SSD_CHUNK, SSD_CHUNK
    xd = (x * dt[..., None]).reshape(b, nc, l, g, j, p)
    bm = bm.reshape(b, nc, l, g, n)
    cm = cm.reshape(b, nc, l, g, n)
    cum = jnp.cumsum((dt * a).reshape(b, nc, l, g, j), axis=2)
    seg = cum[:, :, :, None] - cum[:, :, None, :]
    lower = jnp.tril(jnp.ones((l, l), dtype=bool))[:, :, None, None]
    decay = jnp.exp(jnp.where(lower, seg, -jnp.inf))
    scores = jnp.einsum('bclgn,bcsgn->bclsg', cm, bm)
    y_diag = jnp.einsum('bclsg,bclsgj,bcsgjp->bclgjp', scores, decay, xd)
    end_decay = jnp.exp(cum[:, :, -1:] - cum)
    chunk_states = jnp.einsum('bclgn,bclgj,bclgjp->bcgjpn', bm, end_decay, xd)
    chunk_decay = jnp.exp(cum[:, :, -1])

    def carry_step(state, inp):
        st, dec = inp
        return dec[..., None, None] * state + st, state

    h0 = jnp.zeros((b, g, j, p, n), x.dtype)
    _, h_in = lax.scan(carry_step, h0, (jnp.moveaxis(chunk_states, 1, 0), jnp.moveaxis(chunk_decay, 1, 0)))
    h_in = jnp.moveaxis(h_in, 0, 1)
    y_off = jnp.einsum('bclgn,bcgjpn,bclgj->bclgjp', cm, h_in, jnp.exp(cum))
    return (y_diag + y_off).reshape(b, t, h, p)


def ssd_mixer(z, xbc, dt_raw, lc, conv_w, conv_b, dt_bias, a_log, d_skip, norm_w):
    bsz, t, _ = xbc.shape
    xbc = jnp.concatenate([dwconv_centred(xbc[:, :lc], conv_w, conv_b),
                           dwconv_centred(xbc[:, lc:], conv_w, conv_b)], axis=1)
    xbc = jax.nn.silu(xbc).astype(F32)
    gn = SSD_GROUPS * SSD_STATE
    xs = xbc[..., :SSD_WIDTH].reshape(bsz, t, SSD_HEADS, SSD_HEAD_DIM)
    bm = xbc[..., SSD_WIDTH:SSD_WIDTH + gn].reshape(bsz, t, SSD_GROUPS, SSD_STATE)
    cm = xbc[..., SSD_WIDTH + gn:].reshape(bsz, t, SSD_GROUPS, SSD_STATE)
    dt_raw = dt_raw.astype(F32).reshape(bsz, t, 2, SSD_HEADS)
    y = d_skip.astype(F32)[:, None] * xs
    for d in range(2):
        rev = d == 1
        dt = jax.nn.softplus(dt_raw[:, :, d] + dt_bias[d].astype(F32))
        a = -jnp.exp(a_log[d].astype(F32))
        y_d = ssd_chunked(dir_seq(xs, lc, rev), dir_seq(dt, lc, rev), a,
                          dir_seq(bm, lc, rev), dir_seq(cm, lc, rev))
        y = y + dir_seq(y_d, lc, rev)
    y = y.reshape(bsz, t, SSD_WIDTH)
    return rmsnorm(y * jax.nn.silu(z.astype(F32)), norm_w).astype(z.dtype)


def expert_choice_ffn(x, w_router, w_gate, w_up, w_down):
    bsz, t, d = x.shape
    cap = CAPACITY_FACTOR * t // N_EXPERTS
    aff = jax.nn.softmax(jnp.einsum('btd,de->bte', x, w_router).astype(F32), axis=-1)
    gate, idx = lax.top_k(jnp.swapaxes(aff, 1, 2), cap)
    xs = jax.vmap(lambda xb, ib: xb[ib])(x, idx)
    hid = jax.nn.silu(jnp.einsum('becd,edf->becf', xs, w_gate)) * jnp.einsum('becd,edf->becf', xs, w_up)
    out = jnp.einsum('becf,efd->becd', hid, w_down) * gate[..., None].astype(x.dtype)
    return jax.vmap(lambda ob, ib: jnp.zeros((t, d), ob.dtype).at[ib.reshape(-1)].add(ob.reshape(-1, d)))(out, idx)


def setup_inputs(seed: int = 0) -> dict:
    key = jax.random.key(seed)
    ks = jax.random.split(key, 32)
    L, G, P, H = DEPTH, S5_GROUPS, S5_STATE, S5_GROUP

    def nrm(k, shape, scale):
        return jax.random.normal(k, shape, F32) * scale

    n_idx = jnp.arange(S5_STATE, dtype=F32)
    dt0 = jnp.exp(jax.random.uniform(ks[21], (L, 2, SSD_HEADS), F32, math.log(1e-3), math.log(1e-1)))
    return {
        'x': nrm(ks[0], (BATCH, SEQ, D_MODEL), 1.0),
        'c': nrm(ks[1], (BATCH, D_MODEL), 1.0),
        'ctx': nrm(ks[2], (BATCH, CTX_LEN, D_MODEL), 1.0),
        'c_ctx': nrm(ks[3], (D_MODEL,), 1.0),
        'ada_w': nrm(ks[4], (L, D_MODEL, N_MOD * D_MODEL), 0.2 * D_MODEL ** -0.5),
        'ada_b': nrm(ks[5], (L, N_MOD * D_MODEL), 0.02),
        'norm_g': 1.0 + nrm(ks[6], (L, 4, D_MODEL), 0.02),
        'w_in': nrm(ks[7], (L, D_MODEL, D_IN), D_MODEL ** -0.5),
        'w_out': nrm(ks[8], (L, D_MIX, D_MODEL), D_MIX ** -0.5),
        's5_lam_re': -0.5 + nrm(ks[9], (L, 2, G, P), 0.01),
        's5_lam_im': math.pi * n_idx + nrm(ks[10], (L, 2, G, P), 0.01),
        's5_log_step': jax.random.uniform(ks[11], (L, 2, G), F32, math.log(1e-3), math.log(1e-1)),
        's5_b_re': nrm(ks[12], (L, G, P, H), (2 * H) ** -0.5),
        's5_b_im': nrm(ks[13], (L, G, P, H), (2 * H) ** -0.5),
        's5_c_re': nrm(ks[14], (L, G, H, P), (2 * P) ** -0.5),
        's5_c_im': nrm(ks[15], (L, G, H, P), (2 * P) ** -0.5),
        's5_d': nrm(ks[16], (L, S5_WIDTH), 1.0),
        's5_w_glu': nrm(ks[17], (L, S5_WIDTH, 2 * S5_WIDTH), S5_WIDTH ** -0.5),
        's5_b_glu': nrm(ks[18], (L, 2 * S5_WIDTH), 0.02),
        'ssd_conv_w': nrm(ks[19], (L, SSD_CONV, SSD_XBC), SSD_CONV ** -0.5),
        'ssd_conv_b': nrm(ks[20], (L, SSD_XBC), 0.02),
        'ssd_dt_bias': dt0 + jnp.log(-jnp.expm1(-dt0)),
        'ssd_a_log': jnp.log(jax.random.uniform(ks[22], (L, 2, SSD_HEADS), F32, 1.0, 16.0)),
        'ssd_d': 1.0 + nrm(ks[23], (L, SSD_HEADS), 0.1),
        'ssd_norm': 1.0 + nrm(ks[24], (L, SSD_WIDTH), 0.02),
        'moe_router': nrm(ks[25], (L, D_MODEL, N_EXPERTS), D_MODEL ** -0.5),
        'moe_w_gate': nrm(ks[26], (L, N_EXPERTS, D_MODEL, D_FF), D_MODEL ** -0.5),
        'moe_w_up': nrm(ks[27], (L, N_EXPERTS, D_MODEL, D_FF), D_MODEL ** -0.5),
        'moe_w_down': nrm(ks[28], (L, N_EXPERTS, D_FF, D_MODEL), D_FF ** -0.5),
    }


def reference(x, c, ctx, c_ctx, ada_w, ada_b, norm_g, w_in, w_out, s5_lam_re, s5_lam_im, s5_log_step,
              s5_b_re, s5_b_im, s5_c_re, s5_c_im, s5_d, s5_w_glu, s5_b_glu, ssd_conv_w, ssd_conv_b,
              ssd_dt_bias, ssd_a_log, ssd_d, ssd_norm, moe_router, moe_w_gate, moe_w_up, moe_w_down):
    lc = ctx.shape[1]
    xl, xc = x, ctx
    s1 = S5_WIDTH
    s2 = s1 + SSD_WIDTH
    s3 = s2 + SSD_XBC
    for i in range(DEPTH):
        col_major = i % 2 == 1
        last = i == DEPTH - 1
        mod_l = (jax.nn.silu(c) @ ada_w[i] + ada_b[i])[:, None, :]
        mod_c = (jax.nn.silu(c_ctx) @ ada_w[i] + ada_b[i])[None, None, :]
        sh1_l, sc1_l, g1_l, sh2_l, sc2_l, g2_l = jnp.split(mod_l, N_MOD, axis=-1)
        sh1_c, sc1_c, g1_c, sh2_c, sc2_c, g2_c = jnp.split(mod_c, N_MOD, axis=-1)

        hl = to_scan_order(rmsnorm(xl, norm_g[i, 0]) * (1.0 + sc1_l) + sh1_l, col_major)
        hc = rmsnorm(xc, norm_g[i, 0]) * (1.0 + sc1_c) + sh1_c
        proj = jnp.concatenate([hc, hl], axis=1) @ w_in[i]
        u, z, xbc, dt_raw = proj[..., :s1], proj[..., s1:s2], proj[..., s2:s3], proj[..., s3:]
        y_s5 = s5_mixer(u, lc, s5_lam_re[i], s5_lam_im[i], s5_log_step[i], s5_b_re[i], s5_b_im[i],
                        s5_c_re[i], s5_c_im[i], s5_d[i], s5_w_glu[i], s5_b_glu[i])
        y_ssd = ssd_mixer(z, xbc, dt_raw, lc, ssd_conv_w[i], ssd_conv_b[i], ssd_dt_bias[i],
                          ssd_a_log[i], ssd_d[i], ssd_norm[i])
        mix = jnp.concatenate([y_s5, y_ssd], axis=-1)
        yl = from_scan_order(mix[:, lc:], col_major) @ w_out[i]
        xl = xl + g1_l * rmsnorm(yl, norm_g[i, 1])

        hl2 = rmsnorm(xl, norm_g[i, 2]) * (1.0 + sc2_l) + sh2_l
        yl2 = expert_choice_ffn(hl2, moe_router[i], moe_w_gate[i], moe_w_up[i], moe_w_down[i])
        xl = xl + g2_l * rmsnorm(yl2, norm_g[i, 3])

        if not last:
            yc = mix[:, :lc] @ w_out[i]
            xc = xc + g1_c * rmsnorm(yc, norm_g[i, 1])
            hc2 = rmsnorm(xc, norm_g[i, 2]) * (1.0 + sc2_c) + sh2_c
            yc2 = expert_choice_ffn(hc2, moe_router[i], moe_w_gate[i], moe_w_up[i], moe_w_down[i])
            xc = xc + g2_c * rmsnorm(yc2, norm_g[i, 3])
    return xl
```

```python
import contextlib, numpy as np
import concourse.bass as bass
import concourse.mybir as mybir
from concourse.bass_utils import run_bass_kernel_spmd
F32 = mybir.dt.float32; BF16 = mybir.dt.bfloat16; I32 = mybir.dt.int32; U32 = mybir.dt.uint32
AF = mybir.ActivationFunctionType; ALU = mybir.AluOpType; AX = mybir.AxisListType

NCORES = 8
DEPTH = 4; D = 2048; B = 2; NL = 4096; LC = 256; T = NL + LC; NPOS = B * T
EPS = 1e-6


class Prog:
    def __init__(self):
        self.nc = bass.Bass("TRN2", target_bir_lowering=False)
        self.stack = [contextlib.ExitStack()]
        nc = self.nc
        self.eng = {'pe': nc.tensor, 'act': nc.scalar, 'dve': nc.vector, 'pool': nc.gpsimd, 'sp': nc.sync}
        self.sem = {k: self.stack[0].enter_context(nc.semaphore("s_" + k)) for k in self.eng}
        self.cnt = {k: 0 for k in self.eng}
        self.known = {k: {} for k in self.eng}
        self.lastw = {}
        self.readers = {}
        self.slots = {}
        self.nops = 0
        self.uid = 0

    def dram(self, name, shape, dtype=F32, kind="ExternalInput"):
        return self.nc.dram_tensor(name, list(shape), dtype, kind=kind).ap()

    def sb(self, name, shape, dtype=F32):
        self.uid += 1
        return self.stack[-1].enter_context(self.nc.sbuf_tensor(f"{name}_{self.uid}", list(shape), dtype))

    def ps(self, name, shape, dtype=F32):
        self.uid += 1
        return self.stack[-1].enter_context(self.nc.psum_tensor(f"{name}_{self.uid}", list(shape), dtype))

    def push(self):
        self.stack.append(contextlib.ExitStack())

    def pop(self):
        self.barrier()
        self.stack.pop().close()

    def _wait(self, e, sig):
        if sig is None:
            return
        s, v, key = sig
        if self.known[e].get(key, 0) >= v:
            return
        self.eng[e].wait_ge(s, v)
        self.known[e][key] = v

    def _deps(self, e, r, w):
        for x in r:
            self._wait(e, self.lastw.get(x))
        for x in w:
            self._wait(e, self.lastw.get(x))
            for sg in self.readers.get(x, ()):
                self._wait(e, sg)

    def _commit(self, sig, r, w):
        for x in r:
            self.readers.setdefault(x, []).append(sig)
        for x in w:
            self.lastw[x] = sig
            self.readers[x] = []

    def op(self, e, fn, r=(), w=()):
        self._deps(e, r, w)
        ins = fn(self.eng[e])
        self.cnt[e] += 1
        ins.then_inc(self.sem[e], 1)
        sig = (self.sem[e], self.cnt[e], e)
        if e == 'pe':
            self.known[e][e] = self.cnt[e]
        self._commit(sig, r, w)
        self.nops += 1
        return sig

    def _slot(self, slot):
        if slot not in self.slots:
            self.slots[slot] = [self.stack[0].enter_context(self.nc.semaphore("d_%d" % len(self.slots))), 0]
        return self.slots[slot]

    def dma(self, e, out, in_, r=(), w=(), slot=None, **kw):
        return self.idma(lambda eng: eng.dma_start(out=out, in_=in_, **kw), r, w, slot, e)

    def idma(self, fn, r=(), w=(), slot=None, e='pool'):
        if slot is None:
            slot = "slot_" + str(w[0] if w else r[0])
        sl = self._slot(slot)
        if sl[1] > 0:
            self._wait(e, (sl[0], sl[1], slot))
        self._deps(e, r, w)
        ins = fn(self.eng[e])
        sl[1] += 16
        ins.then_inc(sl[0], 16)
        sig = (sl[0], sl[1], slot)
        self._commit(sig, r, w)
        self.nops += 1
        return sig

    def barrier(self):
        sigs = [(self.sem[k], self.cnt[k], k) for k in self.eng if self.cnt[k] > 0]
        sigs += [(sl[0], sl[1], name) for name, sl in self.slots.items() if sl[1] > 0]
        for e in self.eng:
            for sg in sigs:
                if not (sg[2] == e and e == 'pe'):
                    self._wait(e, sg)

    def finish(self):
        self.barrier()
        return self.nc


def colmajor(v):
    v = np.asarray(v, np.float32)
    lead = v.shape[:-1]
    n = v.shape[-1] // 128
    a = v.reshape(lead + (n, 128))
    a = np.moveaxis(a, -1, 0)
    return np.ascontiguousarray(a)


def build_A():
    p = Prog(); nc = p.nc
    NCOL = 12288 // NCORES
    cT = p.dram("cT", [128, 3, 16])
    adaw = p.dram("adaw", [DEPTH, D, NCOL])
    adab = p.dram("adab", [1, DEPTH * NCOL])
    ones = p.dram("ones", [1, 4])
    mod = p.dram("mod", [3, DEPTH * NCOL], kind="ExternalOutput")
    c_sb = p.sb("c_sb", [128, 3, 16])
    sc = p.sb("sc", [128, 16, 3])
    b_sb = p.sb("b_sb", [1, DEPTH * NCOL])
    one_sb = p.sb("one_sb", [1, 4])
    o_sb = p.sb("o_sb", [3, DEPTH * NCOL])
    wb = [p.sb("wb%d" % i, [128, 16, 512]) for i in range(2)]
    pp = [p.ps("pp%d" % i, [128, 512]) for i in range(2)]
    p.dma('sp', c_sb[:], cT, w=['c_sb'])
    p.dma('sp', b_sb[:], adab, w=['b_sb'])
    p.dma('sp', one_sb[:], ones, w=['one_sb'])
    p.op('act', lambda e: e.activation(out=sc[:].rearrange("p k v -> p v k"), in_=c_sb[:], func=AF.Silu), r=['c_sb'], w=['sc'])
    it = 0
    for l in range(DEPTH):
        for blk in range(NCOL // 512):
            i = it % 2; it += 1
            src = adaw[l, :, blk * 512:(blk + 1) * 512].rearrange("(k p) n -> p k n", p=128)
            p.dma('sp' if i == 0 else 'act', wb[i][:], src, w=['wb%d' % i])
            for ck in range(16):
                p.op('pe', lambda e, ck=ck, i=i: e.matmul(pp[i][0:3, :], lhsT=sc[:, ck, :], rhs=wb[i][:, ck, :],
                                                          start=(ck == 0), stop=False), r=['sc', 'wb%d' % i], w=['pp%d' % i])
            c0 = l * NCOL + blk * 512
            p.op('pe', lambda e, i=i, c0=c0: e.matmul(pp[i][0:3, :], lhsT=one_sb[0:1, 0:3], rhs=b_sb[0:1, c0:c0 + 512],
                                                      start=False, stop=True), r=['one_sb', 'b_sb'], w=['pp%d' % i])
            p.op('act', lambda e, i=i, c0=c0: e.activation(out=o_sb[0:3, c0:c0 + 512], in_=pp[i][0:3, :], func=AF.Copy),
                 r=['pp%d' % i], w=['o_sb'])
    p.dma('sp', mod, o_sb[:], r=['o_sb'], slot='out')
    p.finish()
    return nc


def run_A(inp):
    nc = build_A()
    NCOL = 1536
    cv = np.stack([inp['c'][0], inp['c'][1], inp['c_ctx']], 0).astype(np.float32)
    cT = np.ascontiguousarray(cv.reshape(3, 16, 128).transpose(2, 0, 1))
    maps = []
    for k in range(NCORES):
        maps.append({
            "cT": cT,
            "adaw": np.ascontiguousarray(inp['ada_w'][:, :, k * NCOL:(k + 1) * NCOL]),
            "adab": np.ascontiguousarray(inp['ada_b'][:, k * NCOL:(k + 1) * NCOL]).reshape(1, -1),
            "ones": np.ones((1, 4), np.float32),
        })
    res = run_bass_kernel_spmd(nc, maps, core_ids=list(range(NCORES)))
    mod = np.zeros((DEPTH, 3, 12288), np.float32)
    for k in range(NCORES):
        mod[:, :, k * NCOL:(k + 1) * NCOL] = res.results[k]["mod"].reshape(3, DEPTH, NCOL).transpose(1, 0, 2)
    return mod


L5 = 8
NCH = T // L5
NCHT = B * NCH
NCC = LC // L5
NCL = NL // L5
NT = NPOS // 128
TPB = T // 128
XW = NCH + 1


def cap(t, p0, np_, c0, dims):
    assert p0 == 0
    full = t[:]
    rowstep = full.ap[0][0]
    return bass.AP(full.tensor, full.offset + c0, [[rowstep, np_]] + [list(d) for d in dims])


DBG = {'nblk': 17, 'lvl': 9}


def build_P2(upto=9):
    p = Prog(); nc = p.nc
    xl = p.dram("xl", [B, NL, D]); xc = p.dram("xc", [B, LC, D])
    mc_d = p.dram("mc", [128, 3, 2, 16]); g0_d = p.dram("g0", [128, 16])
    win_d = p.dram("win", [D, 644])
    lam_d = p.dram("lam", [128, 2, 8]); lst_d = p.dram("lst", [128, 8])
    bb_d = p.dram("bb", [128, 2, 4, 16]); cc_d = p.dram("cc", [128, 2, 4, 16]); ds5_d = p.dram("ds5", [128, 8])
    cw_d = p.dram("cw", [128, 3, 5]); cb_d = p.dram("cb", [128, 3]); dtb_d = p.dram("dtb", [128, 4]); alog_d = p.dram("alog", [128, 4])
    dssd_d = p.dram("dssd", [128, 1])
    ident_d = p.dram("ident", [128, 128]); ramps_d = p.dram("ramps", [128, 3, 2, L5])
    maskF_d = p.dram("maskF", [128, 128]); maskB_d = p.dram("maskB", [128, 128])
    triI_d = p.dram("triI", [128, 128]); triIT_d = p.dram("triIT", [128, 128])
    mnF_d = p.dram("mnF", [128, 128]); mnB_d = p.dram("mnB", [128, 128])
    ys5_d = p.dram("ys5", [8, 128, NCHT], kind="ExternalOutput")
    yssd_d = p.dram("yssd", [128, NPOS], kind="ExternalOutput")

    ident = p.sb("ident", [128, 128]); identb = p.sb("identb", [128, 128], BF16)
    ones = p.sb("ones", [128, 128])
    onec = p.sb("onec", [128, 1])
    V = p.sb("V", [128, L5, NCHT], BF16)
    sz = p.sb("sz", [128, NPOS], BF16)
    dtraw = p.sb("dtraw", [128, NT, 4])
    ARENA = 16 * 644 + 2 * 16 * 512 + D
    arena = p.sb("arena", [128, ARENA], BF16)
    p.push()
    xpre = p.sb("xpre", [128, 3, NPOS], BF16)
    p.dma('sp', ident[:], ident_d, w=['ident'])
    p.op('dve', lambda e: e.tensor_copy(out=identb[:], in_=ident[:]), r=['ident'], w=['identb'])
    p.op('dve', lambda e: e.memset(ones[:], 1.0), w=['ones'])
    p.op('dve', lambda e: e.memset(onec[:], 1.0), w=['onec'])

    p.push()
    win = arena[:, 0:16 * 644].rearrange("p (k n) -> p k n", n=644)
    p.dma('pool', win, win_d.rearrange("(k p) n -> p k n", p=128), w=['win'])
    mc = p.sb("mc", [128, 3, 2, 16]); g0 = p.sb("g0", [128, 16]); Gc = p.sb("Gc", [128, 3, 16])
    p.dma('sp', mc[:], mc_d, w=['mc']); p.dma('sp', g0[:], g0_d, w=['g0'])
    for v in range(3):
        p.op('dve', lambda e, v=v: e.scalar_tensor_tensor(out=Gc[:, v, :], in0=mc[:, v, 0, :], scalar=1.0, in1=g0[:],
                                                          op0=ALU.add, op1=ALU.mult), r=['mc', 'g0'], w=['Gc'])
    xtb = [p.sb("xt%d" % i, [128, D]) for i in range(2)]
    junk = arena[:, 16 * 644 + 2 * 8192:16 * 644 + 2 * 8192 + D]
    ss = p.sb("ss", [128, NT]); rs = p.sb("rs", [128, NT])
    hTb = [arena[:, 16 * 644 + i * 8192:16 * 644 + (i + 1) * 8192].rearrange("p (k n) -> p k n", n=512) for i in range(2)]
    ptb = [p.ps("pt%d" % i, [128, 512]) for i in range(2)]
    pmb = [p.ps("pm%d" % i, [128, 512]) for i in range(2)]
    pdt = p.ps("pdt", [128, 512])
    npt = 0; npm = 0
    for blk in range(DBG['nblk']):
        hb = blk % 2; hT = hTb[hb]
        for ti in range(4):
            t = blk * 4 + ti
            b, r = divmod(t, TPB)
            if r < 2:
                src = xc[b, r * 128:(r + 1) * 128, :]; v = 2
            else:
                src = xl[b, (r - 2) * 128:(r - 1) * 128, :]; v = b
            xi = t % 2; xt = xtb[xi]; xn = 'xt%d' % xi
            p.dma('sp' if xi == 0 else 'act', xt[:], src, w=[xn], slot=xn)
            p.op('act', lambda e, xt=xt, t=t: e.activation(out=junk, in_=xt[:], func=AF.Square, accum_out=ss[:, t:t + 1]),
                 r=[xn], w=['junk', ('ss', t)])
            p.op('dve', lambda e, t=t: e.tensor_scalar(out=rs[:, t:t + 1], in0=ss[:, t:t + 1], scalar1=1.0 / D, scalar2=EPS,
                                                       op0=ALU.mult, op1=ALU.add), r=[('ss', t)], w=[('rs', t)])
            p.op('act', lambda e, t=t: e.activation(out=rs[:, t:t + 1], in_=rs[:, t:t + 1], func=AF.Sqrt), r=[('rs', t)], w=[('rs', t)])
            p.op('dve', lambda e, t=t: e.reciprocal(out=rs[:, t:t + 1], in_=rs[:, t:t + 1]), r=[('rs', t)], w=[('rs', t)])
            p.op('dve', lambda e, xt=xt, t=t: e.tensor_scalar(out=xt[:], in0=xt[:], scalar1=rs[:, t:t + 1], scalar2=None, op0=ALU.mult),
                 r=[xn, ('rs', t)], w=[xn])
            if DBG['lvl'] < 2:
                continue
            for q4 in range(4):
                pi = npt % 2; npt += 1; pt = ptb[pi]; pn = 'pt%d' % pi
                for j in range(4):
                    ck = q4 * 4 + j
                    p.op('pe', lambda e, pt=pt, xt=xt, j=j, ck=ck: e.transpose(out=pt[:, j * 128:(j + 1) * 128], in_=xt[:, ck * 128:(ck + 1) * 128],
                                                                               identity=ident[:]), r=[xn, 'ident'], w=[pn])
                for j in range(4):
                    ck = q4 * 4 + j
                    dst = hT[:, ck, ti * 128:(ti + 1) * 128]
                    p.op('act', lambda e, pt=pt, j=j, ck=ck, v=v, dst=dst: e.activation(out=dst, in_=pt[:, j * 128:(j + 1) * 128], func=AF.Identity,
                                                                                        scale=Gc[:, v, ck:ck + 1], bias=mc[:, v, 1, ck:ck + 1]),
                         r=['Gc', 'mc'], w=[pn, ('hT', hb, ti, ck)])
        if DBG['lvl'] < 3:
            continue
        for m in range(5):
            mi = npm % 2; npm += 1; pm = pmb[mi]; mn = 'pm%d' % mi
            for ck in range(16):
                p.op('pe', lambda e, pm=pm, m=m, ck=ck, hT=hT: e.matmul(pm[:], lhsT=win[:, ck, m * 128:(m + 1) * 128], rhs=hT[:, ck, :],
                                                                        start=(ck == 0), stop=(ck == 15)),
                     r=['win'] + [('hT', hb, ti, ck) for ti in range(4)], w=[mn])
            cols = slice(blk * 512, (blk + 1) * 512)
            if m == 0:
                c0 = blk * 64
                p.op('act', lambda e, pm=pm, c0=c0: e.activation(out=V[:, :, c0:c0 + 64], in_=pm[:].rearrange("p (c s) -> p s c", s=L5), func=AF.Copy),
                     w=[mn, ('V', blk)])
            elif m == 1:
                p.op('act', lambda e, pm=pm, cols=cols: e.activation(out=sz[:, cols], in_=pm[:], func=AF.Silu), w=[mn, ('sz', blk)])
            else:
                p.op('dve', lambda e, pm=pm, cols=cols, m=m: e.tensor_copy(out=xpre[:, m - 2, cols], in_=pm[:]), w=[mn, ('xpre', m - 2, blk)])
        if DBG['lvl'] < 4:
            continue
        for ti in range(4):
            for ck in range(16):
                p.op('pe', lambda e, ti=ti, ck=ck, hT=hT: e.matmul(pdt[:, ti * 4:(ti + 1) * 4], lhsT=hT[:, ck, ti * 128:(ti + 1) * 128], rhs=win[:, ck, 640:644],
                                                                   start=(ck == 0), stop=(ck == 15)),
                     r=['win', ('hT', hb, ti, ck)], w=['pdt'])
        p.op('dve', lambda e, blk=blk: e.tensor_copy(out=dtraw[:, blk * 4:(blk + 1) * 4, :], in_=pdt[:, 0:16].rearrange("p (a b) -> p a b", b=4)),
             w=['pdt', ('dtraw', blk)])
    p.pop()
    if upto <= 1:
        p.finish(); return nc

    xs = arena[:, 0:3 * NPOS].rearrange("p (c n) -> p c n", n=NPOS)
    p.push()
    cw = p.sb("cw", [128, 3, 5]); cb = p.sb("cb", [128, 3])
    p.dma('sp', cw[:], cw_d, w=['cw']); p.dma('sp', cb[:], cb_d, w=['cb'])
    p.push()
    accb = [p.sb("acc%d" % i, [128, NL]) for i in range(2)]
    na = 0
    for c3 in range(3):
        for b in range(B):
            for (s0, n) in ((b * T, LC), (b * T + LC, NL)):
                ai = na % 2; na += 1; acc = accb[ai]; an = 'acc%d' % ai
                p.op('act', lambda e, acc=acc, c3=c3, s0=s0, n=n: e.activation(out=acc[:, 0:n], in_=xpre[:, c3, s0:s0 + n], func=AF.Identity,
                                                                               scale=cw[:, c3, 2:3], bias=cb[:, c3:c3 + 1]), r=['cw', 'cb'], w=[an])
                for k in (0, 1, 3, 4):
                    sh = k - 2
                    lo = max(0, -sh); hi = min(n, n - sh)
                    p.op('dve', lambda e, acc=acc, c3=c3, s0=s0, lo=lo, hi=hi, sh=sh, k=k: e.scalar_tensor_tensor(
                        out=acc[:, lo:hi], in0=xpre[:, c3, s0 + lo + sh:s0 + hi + sh], scalar=cw[:, c3, k:k + 1], in1=acc[:, lo:hi],
                        op0=ALU.mult, op1=ALU.add), r=[an, 'cw'], w=[an])
                p.op('act', lambda e, acc=acc, c3=c3, s0=s0, n=n: e.activation(out=xs[:, c3, s0:s0 + n], in_=acc[:, 0:n], func=AF.Silu), r=[an], w=['xs'])
    p.pop()
    p.pop()
    if upto <= 2:
        p.finish(); return nc

    p.push()
    triI = p.sb("triI", [128, 128]); triIT = p.sb("triIT", [128, 128]); mnF = p.sb("mnF", [128, 128]); mnB = p.sb("mnB", [128, 128])
    p.dma('sp', triI[:], triI_d, w=['triI']); p.dma('sp', triIT[:], triIT_d, w=['triIT'])
    p.dma('sp', mnF[:], mnF_d, w=['mnF']); p.dma('sp', mnB[:], mnB_d, w=['mnB'])
    dtb = p.sb("dtb", [128, 4]); alog = p.sb("alog", [128, 4]); dssd = p.sb("dssd", [128, 1])
    p.dma('sp', dtb[:], dtb_d, w=['dtb']); p.dma('sp', alog[:], alog_d, w=['alog']); p.dma('sp', dssd[:], dssd_d, w=['dssd'])
    NQ = NT * 4
    dt_all = p.sb("dt_all", [128, NT, 4]); dA = p.sb("dA", [128, NT, 4]); cum4 = p.sb("cum4", [128, NT, 4]); ncum = p.sb("ncum", [128, NT, 4])
    edec = p.sb("edec", [128, NT, 4]); cdec = p.sb("cdec", [128, NT, 4]); coef = p.sb("coef", [128, NT, 4]); a4 = p.sb("a4", [128, 4])
    bc4 = lambda t: cap(t, 0, 128, 0, [[0, NT], [1, 4]])
    p.op('dve', lambda e: e.tensor_tensor(out=dt_all[:], in0=dtraw[:], in1=bc4(dtb), op=ALU.add), r=['dtb'], w=['dt_all'])
    p.op('act', lambda e: e.activation(out=dt_all[:], in_=dt_all[:], func=AF.Exp), r=['dt_all'], w=['dt_all'])
    p.op('act', lambda e: e.activation(out=dt_all[:], in_=dt_all[:], func=AF.Ln, bias=onec[:], scale=1.0), r=['dt_all', 'onec'], w=['dt_all'])
    p.op('act', lambda e: e.activation(out=a4[:], in_=alog[:], func=AF.Exp), r=['alog'], w=['a4'])
    p.op('dve', lambda e: e.tensor_scalar(out=a4[:], in0=a4[:], scalar1=-1.0, scalar2=None, op0=ALU.mult), r=['a4'], w=['a4'])
    p.op('dve', lambda e: e.tensor_tensor(out=dA[:], in0=dt_all[:], in1=bc4(a4), op=ALU.mult), r=['dt_all', 'a4'], w=['dA'])
    p.push()
    pc = [p.ps("pc%d" % i, [128, 512]) for i in range(3)]
    dAf = dA[:].rearrange("p a b -> p (a b)")
    p.op('pe', lambda e: e.matmul(pc[0][:, 0:NQ], lhsT=triI[:], rhs=dAf, start=True, stop=True), r=['triI', 'dA'], w=['pc0'])
    p.op('pe', lambda e: e.matmul(pc[1][:, 0:NQ], lhsT=triIT[:], rhs=dAf, start=True, stop=True), r=['triIT', 'dA'], w=['pc1'])
    p.op('pe', lambda e: e.matmul(pc[2][:, 0:NQ], lhsT=ones[:], rhs=dAf, start=True, stop=True), r=['ones', 'dA'], w=['pc2'])
    v3 = lambda t: t[:, 0:NQ].rearrange("p (a b) -> p a b", b=4)
    p.op('dve', lambda e: e.tensor_copy(out=cum4[:, :, 0:2], in_=v3(pc[0])[:, :, 0:2]), w=['pc0', 'cum4a'])
    p.op('dve', lambda e: e.tensor_copy(out=cum4[:, :, 2:4], in_=v3(pc[1])[:, :, 2:4]), w=['pc1', 'cum4b'])
    p.op('dve', lambda e: e.tensor_scalar(out=ncum[:], in0=cum4[:], scalar1=-1.0, scalar2=None, op0=ALU.mult), r=['cum4a', 'cum4b'], w=['ncum'])
    p.op('dve', lambda e: e.tensor_tensor(out=edec[:], in0=v3(pc[2]), in1=cum4[:], op=ALU.subtract), r=['cum4a', 'cum4b'], w=['pc2', 'edec'])
    p.op('act', lambda e: e.activation(out=edec[:], in_=edec[:], func=AF.Exp), r=['edec'], w=['edec'])
    p.op('act', lambda e: e.activation(out=cdec[:], in_=v3(pc[2]), func=AF.Exp), w=['pc2', 'cdec'])
    p.op('dve', lambda e: e.tensor_tensor(out=coef[:], in0=dt_all[:], in1=edec[:], op=ALU.mult), r=['dt_all', 'edec'], w=['coef'])
    p.pop()

    ysum = p.sb("ysum", [128, NPOS])
    ptx = [p.ps("ptx%d" % i, [128, 1024], BF16) for i in range(2)]
    psc = p.ps("psc", [128, 512]); pcr = [p.ps("pcr%d" % i, [128, 512]) for i in range(2)]
    psy = p.ps("psy", [128, 512]); pst = p.ps("pst", [128, 512])
    BTs = [p.sb("BTs%d" % i, [128, 128], BF16) for i in range(2)]
    xd = [p.sb("xd%d" % i, [128, 2, 64], BF16) for i in range(2)]
    xde = [p.sb("xde%d" % i, [128, 2, 64], BF16) for i in range(2)]
    rhsj = [p.sb("rhsj%d" % i, [128, 128]) for i in range(2)]
    dec = [p.sb("dec%d" % i, [128, 128]) for i in range(2)]
    ecu = [p.sb("ecu%d" % i, [128, 128]) for i in range(2)]
    Wj = [p.sb("Wj%d" % i, [128, 128], BF16) for i in range(2)]
    Csc = [p.sb("Csc%d" % i, [128, 128], BF16) for i in range(2)]
    hst = p.sb("hst", [128, 2, 64]); hstb = p.sb("hstb", [128, 2, 64], BF16)
    it = 0
    for d in range(2):
        tri_d = triI if d == 0 else triIT
        trin = 'triI' if d == 0 else 'triIT'
        mn_d = mnF if d == 0 else mnB
        mnn = 'mnF' if d == 0 else 'mnB'
        for b in range(B):
            order = list(range(TPB)) if d == 0 else [1, 0] + list(range(TPB - 1, 1, -1))
            for oi, r in enumerate(order):
                first = oi == 0
                t = b * TPB + r
                cols = slice(t * 128, (t + 1) * 128)
                i2 = it % 2; it += 1
                px = ptx[i2]; pxn = 'ptx%d' % i2
                p.op('pe', lambda e, px=px, cols=cols: e.transpose(out=px[:, 0:128], in_=xs[:, 0, cols], identity=identb[:]), r=['xs', 'identb'], w=[pxn])
                p.op('pe', lambda e, px=px, cols=cols: e.transpose(out=px[:, 128:256], in_=xs[:, 1, cols], identity=identb[:]), r=['xs', 'identb'], w=[pxn])
                p.op('act', lambda e, px=px, i2=i2: e.activation(out=BTs[i2][:], in_=px[:, 128:256], func=AF.Copy), w=[pxn, 'BTs%d' % i2])
                p.op('pe', lambda e, cols=cols: e.matmul(psc[:, 0:128], lhsT=xs[:, 1, cols], rhs=xs[:, 2, cols], start=True, stop=True), r=['xs'], w=['psc'])
                pr = pcr[i2]; prn = 'pcr%d' % i2
                for j in range(2):
                    idx = d * 2 + j
                    p.op('dve', lambda e, px=px, j=j, t=t, idx=idx, i2=i2: e.tensor_scalar(out=xd[i2][:, j, :], in0=px[:, j * 64:(j + 1) * 64],
                                                                                          scalar1=dt_all[:, t, idx:idx + 1], scalar2=None, op0=ALU.mult),
                         r=['dt_all'], w=[pxn, ('xd', i2, j)])
                    p.op('act', lambda e, px=px, j=j, t=t, idx=idx, i2=i2: e.activation(out=xde[i2][:, j, :], in_=px[:, j * 64:(j + 1) * 64], func=AF.Copy,
                                                                                        scale=coef[:, t, idx:idx + 1]),
                         r=['coef'], w=[pxn, ('xde', i2, j)])
                    p.op('dve', lambda e, j=j, t=t, idx=idx: e.tensor_scalar(out=rhsj[j][:], in0=tri_d[:], scalar1=dA[:, t, idx:idx + 1], scalar2=None, op0=ALU.mult),
                         r=[trin, 'dA'], w=['rhsj%d' % j])
                    p.op('pe', lambda e, pr=pr, j=j: e.matmul(pr[:, j * 128:(j + 1) * 128], lhsT=ones[:], rhs=rhsj[j][:], start=True, stop=False),
                         r=['ones', 'rhsj%d' % j], w=[prn])
                    p.op('pe', lambda e, pr=pr, j=j: e.matmul(pr[:, j * 128:(j + 1) * 128], lhsT=ident[:], rhs=mn_d[:], start=False, stop=True),
                         r=['ident', mnn], w=[prn])
                    p.op('pe', lambda e, pr=pr, j=j: e.matmul(pr[:, 256 + j * 128:256 + (j + 1) * 128], lhsT=ones[:], rhs=rhsj[j][:], start=True, stop=True),
                         r=['ones', 'rhsj%d' % j], w=[prn])
                    p.op('act', lambda e, pr=pr, j=j, t=t, idx=idx: e.activation(out=dec[j][:], in_=pr[:, j * 128:(j + 1) * 128], func=AF.Exp,
                                                                                bias=ncum[:, t, idx:idx + 1], scale=1.0),
                         r=['ncum'], w=[prn, 'dec%d' % j])
                    p.op('dve', lambda e, j=j: e.tensor_tensor(out=Wj[j][:], in0=psc[:, 0:128], in1=dec[j][:], op=ALU.mult),
                         r=['dec%d' % j], w=['psc', 'Wj%d' % j])
                    p.op('pe', lambda e, j=j, i2=i2: e.matmul(psy[j * 64:(j + 1) * 64, 0:128], lhsT=xd[i2][:, j, :], rhs=Wj[j][:], start=True, stop=first),
                         r=[('xd', i2, j), 'Wj%d' % j], w=['psy'])
                    if not first:
                        p.op('act', lambda e, pr=pr, j=j: e.activation(out=ecu[j][:], in_=pr[:, 256 + j * 128:256 + (j + 1) * 128], func=AF.Exp),
                             w=[prn, 'ecu%d' % j])
                        p.op('pool', lambda e, j=j, cols=cols: e.tensor_tensor(out=Csc[j][:], in0=xs[:, 2, cols], in1=ecu[j][:], op=ALU.mult),
                             r=['xs', 'ecu%d' % j], w=['Csc%d' % j])
                        p.op('pe', lambda e, j=j: e.matmul(psy[j * 64:(j + 1) * 64, 0:128], lhsT=hstb[:, j, :], rhs=Csc[j][:], start=False, stop=True),
                             r=[('hstb', j), 'Csc%d' % j], w=['psy'])
                    p.op('pe', lambda e, j=j, i2=i2: e.matmul(pst[:, j * 64:(j + 1) * 64], lhsT=BTs[i2][:], rhs=xde[i2][:, j, :], start=True, stop=True),
                         r=['BTs%d' % i2, ('xde', i2, j)], w=['pst'])
                    if first:
                        p.op('dve', lambda e, j=j: e.tensor_copy(out=hst[:, j, :], in_=pst[:, j * 64:(j + 1) * 64]), w=['pst', ('hst', j)])
                    else:
                        p.op('dve', lambda e, j=j, t=t, idx=idx: e.scalar_tensor_tensor(out=hst[:, j, :], in0=hst[:, j, :], scalar=cdec[:, t, idx:idx + 1],
                                                                                       in1=pst[:, j * 64:(j + 1) * 64], op0=ALU.mult, op1=ALU.add),
                             r=['cdec'], w=['pst', ('hst', j)])
                    p.op('pool', lambda e, j=j: e.tensor_copy(out=hstb[:, j, :], in_=hst[:, j, :]), r=[('hst', j)], w=[('hstb', j)])
                if d == 0:
                    p.op('dve', lambda e, cols=cols: e.scalar_tensor_tensor(out=ysum[:, cols], in0=xs[:, 0, cols], scalar=dssd[:, 0:1], in1=psy[:, 0:128],
                                                                           op0=ALU.mult, op1=ALU.add),
                         r=['xs', 'dssd'], w=['psy', ('ysum', t)])
                else:
                    p.op('dve', lambda e, cols=cols: e.tensor_tensor(out=ysum[:, cols], in0=psy[:, 0:128], in1=ysum[:, cols], op=ALU.add),
                         w=['psy', ('ysum', t)])
                    p.op('pool', lambda e, cols=cols: e.tensor_tensor(out=ysum[:, cols], in0=ysum[:, cols], in1=sz[:, cols], op=ALU.mult),
                         r=[('ysum', t)], w=[('ysum', t)])
    for q in range(4):
        c = slice(q * (NPOS // 4), (q + 1) * (NPOS // 4))
        p.dma('sp', yssd_d[:, c], ysum[:, c], r=[('ysum', t) for t in range(NT)], slot='yo%d' % q)
    p.pop()
    if upto <= 3:
        p.finish(); return nc

    p.push()
    NDQ = 8
    lam = p.sb("lam", [128, 2, NDQ]); lst = p.sb("lst", [128, NDQ]); bbt = p.sb("bbt", [128, 2, 4, 16]); cct = p.sb("cct", [128, 2, 4, 16])
    ds5 = p.sb("ds5", [128, 8]); ramps = p.sb("ramps", [128, 3, 2, L5]); maskF = p.sb("maskF", [128, 128]); maskB = p.sb("maskB", [128, 128])
    for (tl, dd, nm) in ((lam, lam_d, 'lam'), (lst, lst_d, 'lst'), (bbt, bb_d, 'bbt'), (cct, cc_d, 'cct'), (ds5, ds5_d, 'ds5'), (ramps, ramps_d, 'ramps'),
                         (maskF, maskF_d, 'maskF'), (maskB, maskB_d, 'maskB')):
        p.dma('sp', tl[:], dd, w=[nm])
    U = p.sb("U", [128, 8, NCHT], BF16)
    k = 0
    for g in range(8):
        for s in range(L5):
            p.dma('sp' if k % 2 == 0 else 'act', U[s * 16:(s + 1) * 16, g, :], V[g * 16:(g + 1) * 16, s, :],
                  r=[('V', blk) for blk in range(NPOS // 512)] if k < 8 else [], w=[('U', g)], slot='shuf%d' % (k % 8))
            k += 1
    NSTEP = 10
    Orb = p.sb("Orb", [128, NDQ, 128], BF16); Oib = p.sb("Oib", [128, NDQ, 128], BF16)
    ASrT = p.sb("ASrT", [128, NDQ, 128], BF16); ASiT = p.sb("ASiT", [128, NDQ, 128], BF16)
    Mg = p.sb("Mg", [128, 8, 128], BF16)
    AR = p.sb("AR", [128, NSTEP, NDQ]); AI = p.sb("AI", [128, NSTEP, NDQ]); NAI = p.sb("NAI", [128, NSTEP, NDQ]); tA = p.sb("tA", [128, NDQ])
    p.push()
    st = p.sb("st", [128, NDQ]); a1 = p.sb("a1", [128, NDQ]); an1 = p.sb("an1", [128, NDQ])
    p.op('act', lambda e: e.activation(out=st[:], in_=lst[:], func=AF.Exp), r=['lst'], w=['st'])
    p.op('dve', lambda e: e.tensor_tensor(out=a1[:], in0=lam[:, 0, :], in1=st[:], op=ALU.mult), r=['lam', 'st'], w=['a1'])
    p.op('dve', lambda e: e.tensor_tensor(out=an1[:], in0=lam[:, 1, :], in1=st[:], op=ALU.mult), r=['lam', 'st'], w=['an1'])
    Tre = p.sb("Tre", [128, 3, NDQ, L5]); Tim = p.sb("Tim", [128, 3, NDQ, L5]); mag = p.sb("mag", [128, 3, NDQ, L5])
    arg = p.sb("arg", [128, 3, NDQ, L5]); w1 = p.sb("w1", [128, 3, NDQ, L5]); w2 = p.sb("w2", [128, 3, NDQ, L5]); wi = p.sb("wi", [128, 3, NDQ, L5], I32)
    for i in range(3):
        for d in range(2):
            rb = cap(ramps, 0, 128, (i * 2 + d) * L5, [[0, 4], [1, L5]])
            p.op('dve', lambda e, i=i, d=d, rb=rb: e.tensor_tensor(out=arg[:, i, d * 4:(d + 1) * 4, :], in0=rb, in1=cap(an1, 0, 128, d * 4, [[1, 4], [0, L5]]),
                                                                  op=ALU.mult), r=['ramps', 'an1'], w=['arg'])
            p.op('dve', lambda e, i=i, d=d, rb=rb: e.tensor_tensor(out=mag[:, i, d * 4:(d + 1) * 4, :], in0=rb, in1=cap(a1, 0, 128, d * 4, [[1, 4], [0, L5]]),
                                                                  op=ALU.mult), r=['ramps', 'a1'], w=['mag'])
    p.op('act', lambda e: e.activation(out=mag[:], in_=mag[:], func=AF.Exp), r=['mag'], w=['mag'])
    TWO_PI = float(2 * np.pi)

    def sin_of(dst, off, nm):
        p.op('dve', lambda e: e.tensor_scalar(out=w1[:], in0=arg[:], scalar1=1.0 / TWO_PI, scalar2=64.0 + off, op0=ALU.mult, op1=ALU.add), r=['arg'], w=['w1'])
        p.op('dve', lambda e: e.tensor_copy(out=wi[:], in_=w1[:]), r=['w1'], w=['wi'])
        p.op('dve', lambda e: e.tensor_copy(out=w2[:], in_=wi[:]), r=['wi'], w=['w2'])
        p.op('dve', lambda e: e.tensor_tensor(out=w1[:], in0=w1[:], in1=w2[:], op=ALU.subtract), r=['w1', 'w2'], w=['w1'])
        p.op('dve', lambda e: e.tensor_single_scalar(out=w2[:], in_=w1[:], scalar=0.5, op=ALU.is_ge), r=['w1'], w=['w2'])
        p.op('dve', lambda e: e.tensor_tensor(out=w1[:], in0=w1[:], in1=w2[:], op=ALU.subtract), r=['w1', 'w2'], w=['w1'])
        p.op('act', lambda e: e.activation(out=dst[:], in_=w1[:], func=AF.Sin, scale=TWO_PI), r=['w1'], w=[nm])
    sin_of(Tim, 0.0, 'Tim')
    sin_of(Tre, 0.25, 'Tre')
    p.op('dve', lambda e: e.tensor_tensor(out=Tre[:], in0=Tre[:], in1=mag[:], op=ALU.mult), r=['Tre', 'mag'], w=['Tre'])
    p.op('dve', lambda e: e.tensor_tensor(out=Tim[:], in0=Tim[:], in1=mag[:], op=ALU.mult), r=['Tim', 'mag'], w=['Tim'])
    lb = p.sb("lb", [128, 2, NDQ]); fz = p.sb("fz", [128, 2, NDQ]); tmp8 = p.sb("tmp8", [128, 4, NDQ])
    for d in range(2):
        ix = 0 if d == 0 else L5 - 1
        p.op('dve', lambda e, d=d, ix=ix: e.tensor_copy(out=lb[:, 0, d * 4:(d + 1) * 4], in_=Tre[:, 1, d * 4:(d + 1) * 4, ix]), r=['Tre'], w=['lb'])
        p.op('dve', lambda e, d=d, ix=ix: e.tensor_copy(out=lb[:, 1, d * 4:(d + 1) * 4], in_=Tim[:, 1, d * 4:(d + 1) * 4, ix]), r=['Tim'], w=['lb'])
    nr = tmp8[:, 0, :]; den = tmp8[:, 1, :]; t2 = tmp8[:, 2, :]; t3 = tmp8[:, 3, :]
    T8 = ['tmp8', 'lb', 'lam']
    p.op('dve', lambda e: e.tensor_scalar(out=nr, in0=lb[:, 0, :], scalar1=-1.0, scalar2=None, op0=ALU.add), r=T8, w=['tmp8'])
    p.op('dve', lambda e: e.tensor_tensor(out=den, in0=lam[:, 0, :], in1=lam[:, 0, :], op=ALU.mult), r=T8, w=['tmp8'])
    p.op('dve', lambda e: e.tensor_tensor(out=t2, in0=lam[:, 1, :], in1=lam[:, 1, :], op=ALU.mult), r=T8, w=['tmp8'])
    p.op('dve', lambda e: e.tensor_tensor(out=den, in0=den, in1=t2, op=ALU.add), r=T8, w=['tmp8'])
    p.op('dve', lambda e: e.reciprocal(out=den, in_=den), r=T8, w=['tmp8'])
    p.op('dve', lambda e: e.tensor_tensor(out=t2, in0=nr, in1=lam[:, 0, :], op=ALU.mult), r=T8, w=['tmp8'])
    p.op('dve', lambda e: e.tensor_tensor(out=t3, in0=lb[:, 1, :], in1=lam[:, 1, :], op=ALU.mult), r=T8, w=['tmp8'])
    p.op('dve', lambda e: e.tensor_tensor(out=t2, in0=t2, in1=t3, op=ALU.add), r=T8, w=['tmp8'])
    p.op('dve', lambda e: e.tensor_tensor(out=fz[:, 0, :], in0=t2, in1=den, op=ALU.mult), r=T8, w=['fz'])
    p.op('dve', lambda e: e.tensor_tensor(out=t2, in0=lb[:, 1, :], in1=lam[:, 0, :], op=ALU.mult), r=T8 + ['fz'], w=['tmp8'])
    p.op('dve', lambda e: e.tensor_tensor(out=t3, in0=nr, in1=lam[:, 1, :], op=ALU.mult), r=T8, w=['tmp8'])
    p.op('dve', lambda e: e.tensor_tensor(out=t2, in0=t2, in1=t3, op=ALU.subtract), r=T8, w=['tmp8'])
    p.op('dve', lambda e: e.tensor_tensor(out=fz[:, 1, :], in0=t2, in1=den, op=ALU.mult), r=T8, w=['fz'])
    Bb = p.sb("Bb", [128, 2, NDQ, 16]); tB = p.sb("tB", [128, 4, 16])
    for d in range(2):
        fre = cap(fz, 0, 128, 0 * NDQ + d * 4, [[1, 4], [0, 16]]); fim = cap(fz, 0, 128, 1 * NDQ + d * 4, [[1, 4], [0, 16]])
        dq = slice(d * 4, (d + 1) * 4)
        p.op('dve', lambda e, fre=fre, dq=dq: e.tensor_tensor(out=Bb[:, 0, dq, :], in0=bbt[:, 0], in1=fre, op=ALU.mult), r=['bbt', 'fz'], w=['Bb'])
        p.op('dve', lambda e, fim=fim: e.tensor_tensor(out=tB[:], in0=bbt[:, 1], in1=fim, op=ALU.mult), r=['bbt', 'fz'], w=['tB'])
        p.op('dve', lambda e, dq=dq: e.tensor_tensor(out=Bb[:, 0, dq, :], in0=Bb[:, 0, dq, :], in1=tB[:], op=ALU.subtract), r=['Bb', 'tB'], w=['Bb'])
        p.op('dve', lambda e, fre=fre, dq=dq: e.tensor_tensor(out=Bb[:, 1, dq, :], in0=bbt[:, 1], in1=fre, op=ALU.mult), r=['bbt', 'fz'], w=['Bb'])
        p.op('dve', lambda e, fim=fim: e.tensor_tensor(out=tB[:], in0=bbt[:, 0], in1=fim, op=ALU.mult), r=['bbt', 'fz', 'Bb'], w=['tB'])
        p.op('dve', lambda e, dq=dq: e.tensor_tensor(out=Bb[:, 1, dq, :], in0=Bb[:, 1, dq, :], in1=tB[:], op=ALU.add), r=['Bb', 'tB'], w=['Bb'])
    ASr = p.sb("ASr", [128, NDQ, 128]); ASi = p.sb("ASi", [128, NDQ, 128])
    Or = p.sb("Or", [128, NDQ, 128]); Oi = p.sb("Oi", [128, NDQ, 128])
    Obr = p.sb("Obr", [128, NDQ, 128]); Obi = p.sb("Obi", [128, NDQ, 128])
    tO = p.sb("tO", [128, 2, 128])

    def cprod(outr, outi, ti, src, srcname, dq, q, sq_off, neg_im):
        Pr = cap(Tre, 0, 128, (ti * NDQ + dq) * L5, [[1, L5], [0, 16]]); Pi = cap(Tim, 0, 128, (ti * NDQ + dq) * L5, [[1, L5], [0, 16]])
        nsrc = src.shape[2]
        Xr = cap(src, 0, 128, (0 * nsrc + sq_off) * 16, [[0, L5], [1, 16]]); Xi = cap(src, 0, 128, (1 * nsrc + sq_off) * 16, [[0, L5], [1, 16]])
        o_r = outr[:, dq, :].rearrange("p (s h) -> p s h", h=16); o_i = outi[:, dq, :].rearrange("p (s h) -> p s h", h=16)
        t0 = tO[:, 0, :].rearrange("p (s h) -> p s h", h=16); t1 = tO[:, 1, :].rearrange("p (s h) -> p s h", h=16)
        R = ['Tre', 'Tim', srcname, 'tO']
        p.op('dve', lambda e: e.tensor_tensor(out=o_r, in0=Pr, in1=Xr, op=ALU.mult), r=R, w=['opr'])
        p.op('dve', lambda e: e.tensor_tensor(out=t0, in0=Pi, in1=Xi, op=ALU.mult), r=R, w=['tO'])
        p.op('dve', lambda e: e.tensor_tensor(out=o_r, in0=o_r, in1=t0, op=ALU.subtract), r=R + ['opr'], w=['opr'])
        p.op('dve', lambda e: e.tensor_tensor(out=o_i, in0=Pr, in1=Xi, op=ALU.mult), r=R, w=['opi'])
        p.op('dve', lambda e: e.tensor_tensor(out=t1, in0=Pi, in1=Xr, op=ALU.mult), r=R, w=['tO'])
        if neg_im:
            p.op('dve', lambda e: e.scalar_tensor_tensor(out=o_i, in0=o_i, scalar=-1.0, in1=t1, op0=ALU.mult, op1=ALU.subtract), r=R + ['opi'], w=['opi'])
        else:
            p.op('dve', lambda e: e.tensor_tensor(out=o_i, in0=o_i, in1=t1, op=ALU.add), r=R + ['opi'], w=['opi'])
    for dq in range(NDQ):
        q = dq % 4
        cprod(ASr, ASi, 0, Bb, 'Bb', dq, q, dq, False)
        cprod(Or, Oi, 1, cct, 'cct', dq, q, q, True)
        cprod(Obr, Obi, 2, cct, 'cct', dq, q, q, True)
    p.op('act', lambda e: e.activation(out=Orb[:], in_=Or[:], func=AF.Copy), r=['opr', 'opi'], w=['Orb'])
    p.op('act', lambda e: e.activation(out=Oib[:], in_=Oi[:], func=AF.Copy), r=['opr', 'opi'], w=['Oib'])
    p.push()
    ptr = [p.ps("ptr%d" % i, [128, 512]) for i in range(2)]
    pM = [p.ps("pM%d" % i, [128, 512]) for i in range(2)]
    tM = [p.sb("tM%d" % i, [128, 128]) for i in range(2)]
    for dq in range(NDQ):
        i2 = dq % 2
        p.op('pe', lambda e, dq=dq, i2=i2: e.transpose(out=ptr[i2][:, 0:128], in_=ASr[:, dq, :], identity=ident[:]), r=['opr', 'opi', 'ident'], w=['ptr%d' % i2])
        p.op('pe', lambda e, dq=dq, i2=i2: e.transpose(out=ptr[i2][:, 128:256], in_=ASi[:, dq, :], identity=ident[:]), r=['opr', 'opi', 'ident'], w=['ptr%d' % i2])
        p.op('act', lambda e, dq=dq, i2=i2: e.activation(out=ASrT[:, dq, :], in_=ptr[i2][:, 0:128], func=AF.Copy), w=['ptr%d' % i2, 'ASrT'])
        p.op('act', lambda e, dq=dq, i2=i2: e.activation(out=ASiT[:, dq, :], in_=ptr[i2][:, 128:256], func=AF.Copy), w=['ptr%d' % i2, 'ASiT'])
    for g in range(8):
        q, m = divmod(g, 2)
        rows = slice(m * 64, (m + 1) * 64)
        for d in range(2):
            dq = d * 4 + q
            p.op('pe', lambda e, d=d, dq=dq, rows=rows: e.matmul(pM[d][:, 0:128], lhsT=ASr[rows, dq, :], rhs=Obr[rows, dq, :], start=True, stop=False),
                 r=['opr', 'opi'], w=['pM%d' % d])
            p.op('pe', lambda e, d=d, dq=dq, rows=rows: e.matmul(pM[d][:, 0:128], lhsT=ASi[rows, dq, :], rhs=Obi[rows, dq, :], start=False, stop=True),
                 r=['opr', 'opi'], w=['pM%d' % d])
        p.op('dve', lambda e: e.tensor_tensor(out=tM[0][:], in0=pM[0][:, 0:128], in1=maskF[:], op=ALU.mult), r=['maskF'], w=['pM0', 'tM0'])
        p.op('dve', lambda e: e.tensor_tensor(out=tM[1][:], in0=pM[1][:, 0:128], in1=maskB[:], op=ALU.mult), r=['maskB'], w=['pM1', 'tM1'])
        p.op('dve', lambda e: e.tensor_tensor(out=tM[0][:], in0=tM[0][:], in1=tM[1][:], op=ALU.add), r=['tM0', 'tM1'], w=['tM0'])
        p.op('dve', lambda e, g=g: e.scalar_tensor_tensor(out=Mg[:, g, :], in0=ident[:], scalar=ds5[:, g:g + 1], in1=tM[0][:], op0=ALU.mult, op1=ALU.add),
             r=['tM0', 'ident', 'ds5'], w=[('Mg', g)])
    p.pop()
    for d in range(2):
        ix = L5 - 1 if d == 0 else 0
        p.op('dve', lambda e, d=d, ix=ix: e.tensor_copy(out=AR[:, 0, d * 4:(d + 1) * 4], in_=Tre[:, 1, d * 4:(d + 1) * 4, ix]), r=['Tre'], w=['AR'])
        p.op('dve', lambda e, d=d, ix=ix: e.tensor_copy(out=AI[:, 0, d * 4:(d + 1) * 4], in_=Tim[:, 1, d * 4:(d + 1) * 4, ix]), r=['Tim'], w=['AI'])
    RA = ['AR', 'AI', 'tA']
    for k in range(1, NSTEP):
        p.op('dve', lambda e, k=k: e.tensor_tensor(out=AR[:, k, :], in0=AR[:, k - 1, :], in1=AR[:, k - 1, :], op=ALU.mult), r=RA, w=['AR'])
        p.op('dve', lambda e, k=k: e.tensor_tensor(out=tA[:], in0=AI[:, k - 1, :], in1=AI[:, k - 1, :], op=ALU.mult), r=RA, w=['tA'])
        p.op('dve', lambda e, k=k: e.tensor_tensor(out=AI[:, k, :], in0=AR[:, k - 1, :], in1=AI[:, k - 1, :], op=ALU.mult), r=RA, w=['AI'])
        p.op('dve', lambda e, k=k: e.tensor_tensor(out=AR[:, k, :], in0=AR[:, k, :], in1=tA[:], op=ALU.subtract), r=RA, w=['AR'])
        p.op('dve', lambda e, k=k: e.tensor_scalar(out=AI[:, k, :], in0=AI[:, k, :], scalar1=2.0, scalar2=None, op0=ALU.mult), r=RA, w=['AI'])
    p.op('dve', lambda e: e.tensor_scalar(out=NAI[:], in0=AI[:], scalar1=-1.0, scalar2=None, op0=ALU.mult), r=RA, w=['NAI'])
    p.pop()

    pS = [p.ps("pS%d" % i, [128, 512]) for i in range(4)]
    pY = [p.ps("pY%d" % i, [128, 512]) for i in range(2)]
    Hpp = [[p.sb("Hp%d%d" % (c, z), [128, B, XW]) for z in range(2)] for c in range(2)]
    H16 = [[p.sb("H16%d%d" % (d, c), [128, B, XW], BF16) for c in range(2)] for d in range(2)]
    yo = [p.sb("yo%d" % i, [128, 512]) for i in range(2)]
    segs = []
    for b in range(B):
        segs.append((b, 0, b * NCH, NCC))
        segs.append((b, 1, b * NCH + NCC, NCL))
    nS = 0; nY = 0
    LASTK = max(k for k in range(NSTEP) if (1 << k) < XW)
    for q in range(4):
        for d in range(2):
            dq = d * 4 + q
            hn = lambda c, z: 'Hp%d%d' % (c, z)
            X0r, X0i = Hpp[0][0], Hpp[1][0]
            zc = 0 if d == 0 else NCH
            for c in range(2):
                p.op('pool', lambda e, c=c, zc=zc: e.memset(Hpp[c][0][:, :, zc:zc + 1], 0.0), w=[hn(c, 0)])
            for (b, il, c0, n) in segs:
                si = nS % 2; nS += 1
                pr_, pi_ = pS[si * 2], pS[si * 2 + 1]
                for m in range(2):
                    g = q * 2 + m
                    rows = slice(m * 64, (m + 1) * 64)
                    p.op('pe', lambda e, pr_=pr_, dq=dq, rows=rows, g=g, c0=c0, n=n: e.matmul(pr_[rows, 0:n], lhsT=ASrT[:, dq, rows], rhs=U[:, g, c0:c0 + n], start=True, stop=True),
                         r=['ASrT', ('U', g)], w=['pSr%d' % si])
                    p.op('pe', lambda e, pi_=pi_, dq=dq, rows=rows, g=g, c0=c0, n=n: e.matmul(pi_[rows, 0:n], lhsT=ASiT[:, dq, rows], rhs=U[:, g, c0:c0 + n], start=True, stop=True),
                         r=['ASiT', ('U', g)], w=['pSi%d' % si])
                if d == 0:
                    dst = 1 + (NCC if il else 0)
                else:
                    dst = 0 if il else NCL
                p.op('act', lambda e, pr_=pr_, b=b, dst=dst, n=n: e.activation(out=X0r[:, b, dst:dst + n], in_=pr_[:, 0:n], func=AF.Copy),
                     w=['pSr%d' % si, hn(0, 0)])
                p.op('act', lambda e, pi_=pi_, b=b, dst=dst, n=n: e.activation(out=X0i[:, b, dst:dst + n], in_=pi_[:, 0:n], func=AF.Copy),
                     w=['pSi%d' % si, hn(1, 0)])
            cur = 0
            for k in range(LASTK + 1):
                sh = 1 << k
                nxt = 1 - cur
                Rr, Ri = Hpp[0][cur], Hpp[1][cur]
                if k == LASTK:
                    Wr, Wi = H16[d][0], H16[d][1]
                    wn0, wn1 = 'H16%d0' % d, 'H16%d1' % d
                else:
                    Wr, Wi = Hpp[0][nxt], Hpp[1][nxt]
                    wn0, wn1 = hn(0, nxt), hn(1, nxt)
                if d == 0:
                    dsl = slice(sh, XW); ssl = slice(0, XW - sh); keep = slice(0, sh)
                else:
                    dsl = slice(0, XW - sh); ssl = slice(sh, XW); keep = slice(XW - sh, XW)
                ar = AR[:, k, dq:dq + 1]; ai = AI[:, k, dq:dq + 1]; nai = NAI[:, k, dq:dq + 1]
                rr = [hn(0, cur), hn(1, cur), 'AR', 'AI', 'NAI']
                p.op('pool', lambda e, Wr=Wr, Rr=Rr, keep=keep: e.tensor_copy(out=Wr[:, :, keep], in_=Rr[:, :, keep]), r=rr, w=[wn0])
                p.op('pool', lambda e, Wi=Wi, Ri=Ri, keep=keep: e.tensor_copy(out=Wi[:, :, keep], in_=Ri[:, :, keep]), r=rr, w=[wn1])
                p.op('dve', lambda e, Wr=Wr, Rr=Rr, ar=ar, dsl=dsl, ssl=ssl: e.scalar_tensor_tensor(out=Wr[:, :, dsl], in0=Rr[:, :, ssl], scalar=ar, in1=Rr[:, :, dsl],
                                                                                                   op0=ALU.mult, op1=ALU.add), r=rr, w=[wn0])
                p.op('dve', lambda e, Wr=Wr, Ri=Ri, nai=nai, dsl=dsl, ssl=ssl: e.scalar_tensor_tensor(out=Wr[:, :, dsl], in0=Ri[:, :, ssl], scalar=nai, in1=Wr[:, :, dsl],
                                                                                                     op0=ALU.mult, op1=ALU.add), r=rr + [wn0], w=[wn0])
                p.op('dve', lambda e, Wi=Wi, Ri=Ri, ar=ar, dsl=dsl, ssl=ssl: e.scalar_tensor_tensor(out=Wi[:, :, dsl], in0=Ri[:, :, ssl], scalar=ar, in1=Ri[:, :, dsl],
                                                                                                   op0=ALU.mult, op1=ALU.add), r=rr, w=[wn1])
                p.op('dve', lambda e, Wi=Wi, Rr=Rr, ai=ai, dsl=dsl, ssl=ssl: e.scalar_tensor_tensor(out=Wi[:, :, dsl], in0=Rr[:, :, ssl], scalar=ai, in1=Wi[:, :, dsl],
                                                                                                   op0=ALU.mult, op1=ALU.add), r=rr + [wn1], w=[wn1])
                cur = nxt
        for m in range(2):
            g = q * 2 + m
            rows = slice(m * 64, (m + 1) * 64)
            for (b, il, c0, n) in segs:
                yi = nY % 2; nY += 1; py = pY[yi]; pyn = 'pY%d' % yi
                p.op('pe', lambda e, py=py, g=g, c0=c0, n=n: e.matmul(py[:, 0:n], lhsT=Mg[:, g, :], rhs=U[:, g, c0:c0 + n], start=True, stop=False),
                     r=[('Mg', g), ('U', g)], w=[pyn])
                for d in range(2):
                    dq = d * 4 + q
                    if d == 0:
                        off = NCC if il else 0
                    else:
                        off = 1 if il else NCL + 1
                    for c in range(2):
                        hv = H16[d][c]; hname = 'H16%d%d' % (d, c)
                        Ob_ = Orb if c == 0 else Oib
                        last = (d == 1 and c == 1)
                        p.op('pe', lambda e, py=py, Ob_=Ob_, rows=rows, dq=dq, hv=hv, b=b, off=off, n=n, last=last: e.matmul(
                            py[:, 0:n], lhsT=Ob_[rows, dq, :], rhs=hv[rows, b, off:off + n], start=False, stop=last),
                            r=['Orb', 'Oib', hname], w=[pyn])
                p.op('act', lambda e, py=py, yi=yi, n=n: e.activation(out=yo[yi][:, 0:n], in_=py[:, 0:n], func=AF.Gelu_apprx_tanh), w=[pyn, 'yo%d' % yi])
                p.dma('sp', ys5_d[g, :, c0:c0 + n], yo[yi][:, 0:n], r=['yo%d' % yi], slot='ys%d' % yi)
    p.pop()
    p.finish()
    return nc


def p2_consts():
    c = {}
    c["ident"] = np.eye(128, dtype=np.float32)
    L = L5
    r = np.zeros((3, 2, L), np.float32)
    s = np.arange(L, dtype=np.float32)
    r[0, 0] = L - 1 - s; r[0, 1] = s
    r[1, 0] = s + 1; r[1, 1] = L - s
    r[2, 0] = s - (L - 1); r[2, 1] = -s
    c["ramps"] = np.ascontiguousarray(np.broadcast_to(r[None], (128, 3, 2, L)))
    sidx = np.arange(128) // 16
    c["maskF"] = (sidx[None, :] >= sidx[:, None]).astype(np.float32)
    c["maskB"] = (sidx[None, :] <= sidx[:, None]).astype(np.float32)
    k = np.arange(128)
    c["triI"] = (k[:, None] <= k[None, :]).astype(np.float32)
    c["triIT"] = np.ascontiguousarray(c["triI"].T)
    c["mnF"] = np.where(k[None, :] >= k[:, None], 0.0, -30000.0).astype(np.float32)
    c["mnB"] = np.where(k[None, :] <= k[:, None], 0.0, -30000.0).astype(np.float32)
    return c


def p2_inputs(inp, l, k, xl, xc, mod_l):
    m = {}
    m["xl"] = xl; m["xc"] = xc
    sh1 = mod_l[:, 0:2048]; sc1 = mod_l[:, 2048:4096]
    mc = np.stack([colmajor(sc1), colmajor(sh1)], axis=2)
    m["mc"] = np.ascontiguousarray(mc)
    m["g0"] = colmajor(inp['norm_g'][l, 0])
    s1, s2 = 1024, 2048
    gg = k // 4
    cols = np.concatenate([np.arange(k * 128, (k + 1) * 128), s1 + np.arange(k * 128, (k + 1) * 128), s2 + np.arange(k * 128, (k + 1) * 128),
                           s2 + 1024 + gg * 128 + np.arange(128), s2 + 1024 + 256 + gg * 128 + np.arange(128),
                           s2 + 1536 + np.array([2 * k, 2 * k + 1, 16 + 2 * k, 16 + 2 * k + 1])])
    m["win"] = np.ascontiguousarray(inp['w_in'][l][:, cols])
    G0 = 8 * k
    lam = np.zeros((128, 2, 8), np.float32); lst = np.zeros((128, 8), np.float32)
    bb = np.zeros((128, 2, 4, 16), np.float32); cc = np.zeros((128, 2, 4, 16), np.float32)
    for q in range(4):
        for mm in range(2):
            g = G0 + 2 * q + mm
            rows = slice(mm * 64, (mm + 1) * 64)
            for d in range(2):
                lam[rows, 0, d * 4 + q] = inp['s5_lam_re'][l, d, g]
                lam[rows, 1, d * 4 + q] = inp['s5_lam_im'][l, d, g]
                lst[rows, d * 4 + q] = inp['s5_log_step'][l, d, g]
            bb[rows, 0, q] = inp['s5_b_re'][l, g]; bb[rows, 1, q] = inp['s5_b_im'][l, g]
            cc[rows, 0, q] = inp['s5_c_re'][l, g].T; cc[rows, 1, q] = inp['s5_c_im'][l, g].T
    m["lam"] = lam; m["lst"] = lst; m["bb"] = bb; m["cc"] = cc
    dd = inp['s5_d'][l][k * 128:(k + 1) * 128].reshape(8, 16)
    m["ds5"] = np.ascontiguousarray(np.broadcast_to(dd.T[None], (L5, 16, 8)).reshape(128, 8))
    ch = np.concatenate([k * 128 + np.arange(128), 1024 + gg * 128 + np.arange(128), 1024 + 256 + gg * 128 + np.arange(128)])
    m["cw"] = np.ascontiguousarray(inp['ssd_conv_w'][l][:, ch].reshape(5, 3, 128).transpose(2, 1, 0))
    m["cb"] = np.ascontiguousarray(inp['ssd_conv_b'][l][ch].reshape(3, 128).T)
    hd = [2 * k, 2 * k + 1]
    dtb = np.array([inp['ssd_dt_bias'][l, d, h] for d in range(2) for h in hd], np.float32)
    alg = np.array([inp['ssd_a_log'][l, d, h] for d in range(2) for h in hd], np.float32)
    m["dtb"] = np.ascontiguousarray(np.broadcast_to(dtb[None], (128, 4))); m["alog"] = np.ascontiguousarray(np.broadcast_to(alg[None], (128, 4)))
    m["dssd"] = np.ascontiguousarray(np.repeat(inp['ssd_d'][l][hd], 64).reshape(128, 1).astype(np.float32))
    m.update(p2_consts())
    return m

NTOK3 = 1088
NB3 = 256


def build_P3():
    p = Prog(); nc = p.nc
    mix_d = p.dram("mixin", [128, 16, NTOK3]); xT_d = p.dram("xT", [128, 16, NTOK3])
    wglu_d = p.dram("wglu", [1024, 2048]); bglu_d = p.dram("bglu", [128, 16])
    wout_d = p.dram("wout", [2048, 2048]); ssdn_d = p.dram("ssdn", [128, 8])
    mcol_d = p.dram("mcol", [128, 2, 3, 16]); ng_d = p.dram("ng", [128, 2, 16])
    wr_d = p.dram("wr", [128, 16, 16])
    x1_d = p.dram("x1T", [128, 16, NTOK3], kind="ExternalOutput")
    h2_d = p.dram("h2T", [128, 16, NTOK3], kind="ExternalOutput")
    aff_d = p.dram("aff", [NTOK3, 16], kind="ExternalOutput")

    wglu = p.sb("wglu", [128, 8, 2048], BF16); wout = p.sb("wout", [128, 16, 2048], BF16)
    p.dma('pool', wglu[:], wglu_d.rearrange("(k p) n -> p k n", p=128), w=['wglu'])
    p.dma('pool', wout[:], wout_d.rearrange("(k p) n -> p k n", p=128), w=['wout'])
    bglu = p.sb("bglu", [128, 16]); ssdn = p.sb("ssdn", [128, 8]); mcol = p.sb("mcol", [128, 2, 3, 16]); ng = p.sb("ng", [128, 2, 16])
    wr = p.sb("wr", [128, 16, 16]); ones = p.sb("ones", [128, 128])
    for (tl, dd, nm) in ((bglu, bglu_d, 'bglu'), (ssdn, ssdn_d, 'ssdn'), (mcol, mcol_d, 'mcol'), (ng, ng_d, 'ng'), (wr, wr_d, 'wr')):
        p.dma('sp', tl[:], dd, w=[nm])
    p.op('dve', lambda e: e.memset(ones[:], 1.0), w=['ones'])
    cA = p.sb("cA", [128, 2, 16]); G2 = p.sb("G2", [128, 2, 16])
    for v in range(2):
        p.op('dve', lambda e, v=v: e.tensor_tensor(out=cA[:, v, :], in0=mcol[:, v, 0, :], in1=ng[:, 0, :], op=ALU.mult), r=['mcol', 'ng'], w=['cA'])
        p.op('dve', lambda e, v=v: e.scalar_tensor_tensor(out=G2[:, v, :], in0=mcol[:, v, 1, :], scalar=1.0, in1=ng[:, 1, :], op0=ALU.add, op1=ALU.mult),
             r=['mcol', 'ng'], w=['G2'])
    mix = p.sb("mix", [128, 16, NB3]); ys5b = p.sb("ys5b", [128, 8, NB3], BF16); mixb = p.sb("mixb", [128, 16, NB3], BF16)
    yl = p.sb("yl", [128, 16, NB3]); xt = p.sb("xt", [128, 16, NB3]); h2 = p.sb("h2", [128, 16, NB3])
    sq = [p.sb("sq%d" % i, [128, NB3]) for i in range(2)]
    sig = [p.sb("sig%d" % i, [128, NB3]) for i in range(2)]
    rstd = p.sb("rstd", [128, NB3])
    lg = p.sb("lg", [128, 16]); mx = p.sb("mx", [128, 1]); sm = p.sb("sm", [128, 1]); ex = p.sb("ex", [128, 16]); af = p.sb("af", [128, 16])
    psA = p.ps("psA", [128, 512]); psB = p.ps("psB", [128, 512]); pss = p.ps("pss", [128, 512])
    pyl = [p.ps("pyl%d" % i, [128, 512]) for i in range(2)]; prt = p.ps("prt", [128, 512])

    def norm_rstd(src, nch, n, dim, tag):
        for k in range(nch):
            s_ = sq[k % 2]; sn = 'sq%d' % (k % 2)
            p.op('pool' if k % 2 else 'dve', lambda e, s_=s_, k=k: e.tensor_tensor(out=s_[:, 0:n], in0=src[:, k, 0:n], in1=src[:, k, 0:n], op=ALU.mult),
                 r=[tag], w=[sn])
            p.op('pe', lambda e, s_=s_, k=k: e.matmul(pss[:, 0:n], lhsT=ones[:], rhs=s_[:, 0:n], start=(k == 0), stop=(k == nch - 1)),
                 r=['ones', sn], w=['pss'])
        p.op('dve', lambda e: e.tensor_scalar(out=rstd[:, 0:n], in0=pss[:, 0:n], scalar1=1.0 / dim, scalar2=EPS, op0=ALU.mult, op1=ALU.add), w=['pss', 'rstd'])
        p.op('act', lambda e: e.activation(out=rstd[:, 0:n], in_=rstd[:, 0:n], func=AF.Sqrt), w=['rstd'])
        p.op('dve', lambda e: e.reciprocal(out=rstd[:, 0:n], in_=rstd[:, 0:n]), w=['rstd'])

    blocks = [(i * NB3, NB3, 0) for i in range(1024 // NB3)] + [(1024, 64, 1)]
    nyl = 0
    for (t0, n, v) in blocks:
        cs = slice(t0, t0 + n)
        p.dma('sp', mix[:, :, 0:n], mix_d[:, :, cs], w=['mix'], slot='mix')
        p.dma('act', xt[:, :, 0:n], xT_d[:, :, cs], w=['xt'], slot='xt')
        p.op('act', lambda e: e.activation(out=ys5b[:, :, 0:n], in_=mix[:, 0:8, 0:n], func=AF.Copy), r=['mix'], w=['ys5b'])
        for mo in range(8):
            for k in range(8):
                p.op('pe', lambda e, mo=mo, k=k: e.matmul(psA[:, 0:n], lhsT=wglu[:, k, mo * 128:(mo + 1) * 128], rhs=ys5b[:, k, 0:n], start=(k == 0), stop=(k == 7)),
                     r=['wglu', 'ys5b'], w=['psA'])
            for k in range(8):
                p.op('pe', lambda e, mo=mo, k=k: e.matmul(psB[:, 0:n], lhsT=wglu[:, k, (8 + mo) * 128:(9 + mo) * 128], rhs=ys5b[:, k, 0:n], start=(k == 0), stop=(k == 7)),
                     r=['wglu', 'ys5b'], w=['psB'])
            sg = sig[mo % 2]; sgn = 'sig%d' % (mo % 2)
            p.op('act', lambda e, mo=mo, sg=sg: e.activation(out=sg[:, 0:n], in_=psB[:, 0:n], func=AF.Sigmoid, bias=bglu[:, 8 + mo:9 + mo], scale=1.0),
                 r=['bglu'], w=['psB', sgn])
            p.op('dve', lambda e, mo=mo, sg=sg: e.scalar_tensor_tensor(out=mixb[:, mo, 0:n], in0=psA[:, 0:n], scalar=bglu[:, mo:mo + 1], in1=sg[:, 0:n],
                                                                      op0=ALU.add, op1=ALU.mult), r=['bglu', sgn], w=['psA', ('mixb', mo)])
        norm_rstd(mix[:, 8:16, :], 8, n, 1024.0, 'mix')
        for k in range(8):
            p.op('dve', lambda e, k=k: e.scalar_tensor_tensor(out=mixb[:, 8 + k, 0:n], in0=mix[:, 8 + k, 0:n], scalar=ssdn[:, k:k + 1], in1=rstd[:, 0:n],
                                                                                  op0=ALU.mult, op1=ALU.mult), r=['mix', 'ssdn', 'rstd'], w=[('mixb', 8 + k)])
        for mo in range(16):
            yi = nyl % 2; nyl += 1; py = pyl[yi]; pyn = 'pyl%d' % yi
            for k in range(16):
                p.op('pe', lambda e, py=py, mo=mo, k=k: e.matmul(py[:, 0:n], lhsT=wout[:, k, mo * 128:(mo + 1) * 128], rhs=mixb[:, k, 0:n], start=(k == 0), stop=(k == 15)),
                     r=['wout', ('mixb', k)], w=[pyn])
            p.op('act', lambda e, py=py, mo=mo: e.activation(out=yl[:, mo, 0:n], in_=py[:, 0:n], func=AF.Copy), w=[pyn, 'yl'])
        norm_rstd(yl, 16, n, float(D), 'yl')
        for mo in range(16):
            eng = 'pool'
            p.op('dve', lambda e, mo=mo: e.scalar_tensor_tensor(out=yl[:, mo, 0:n], in0=yl[:, mo, 0:n], scalar=cA[:, v, mo:mo + 1], in1=rstd[:, 0:n],
                                                             op0=ALU.mult, op1=ALU.mult), r=['cA', 'rstd'], w=['yl'])
            p.op(eng, lambda e, mo=mo: e.tensor_tensor(out=xt[:, mo, 0:n], in0=xt[:, mo, 0:n], in1=yl[:, mo, 0:n], op=ALU.add), r=['yl'], w=['xt'])
        p.dma('sp', x1_d[:, :, cs], xt[:, :, 0:n], r=['xt'], slot='x1o')
        norm_rstd(xt, 16, n, float(D), 'xt')
        for mo in range(16):
            p.op('dve', lambda e, mo=mo: e.scalar_tensor_tensor(out=h2[:, mo, 0:n], in0=xt[:, mo, 0:n], scalar=G2[:, v, mo:mo + 1], in1=rstd[:, 0:n],
                                                                                     op0=ALU.mult, op1=ALU.mult), r=['xt', 'G2', 'rstd'], w=['h2'])
        for mo in range(16):
            p.op('act', lambda e, mo=mo: e.activation(out=h2[:, mo, 0:n], in_=h2[:, mo, 0:n], func=AF.Identity, bias=mcol[:, v, 2, mo:mo + 1], scale=1.0),
                 r=['mcol'], w=['h2'])
        p.dma('sp', h2_d[:, :, cs], h2[:, :, 0:n], r=['h2'], slot='h2o')
        for ti in range((n + 127) // 128):
            m = min(128, n - ti * 128)
            for k in range(16):
                p.op('pe', lambda e, ti=ti, k=k, m=m: e.matmul(prt[0:m, 0:16], lhsT=h2[:, k, ti * 128:ti * 128 + m], rhs=wr[:, k, :], start=(k == 0), stop=(k == 15)),
                     r=['h2', 'wr'], w=['prt'])
            p.op('act', lambda e, m=m: e.activation(out=lg[0:m, :], in_=prt[0:m, 0:16], func=AF.Copy), w=['prt', 'lg'])
            p.op('dve', lambda e, m=m: e.tensor_reduce(out=mx[0:m, :], in_=lg[0:m, :], axis=AX.X, op=ALU.max), r=['lg'], w=['mx'])
            p.op('dve', lambda e, m=m: e.tensor_scalar(out=mx[0:m, :], in0=mx[0:m, :], scalar1=-1.0, scalar2=None, op0=ALU.mult), w=['mx'])
            p.op('act', lambda e, m=m: e.activation(out=ex[0:m, :], in_=lg[0:m, :], func=AF.Exp, bias=mx[0:m, :], scale=1.0, accum_out=sm[0:m, :]),
                 r=['lg', 'mx'], w=['ex', 'sm'])
            p.op('dve', lambda e, m=m: e.reciprocal(out=sm[0:m, :], in_=sm[0:m, :]), w=['sm'])
            p.op('dve', lambda e, m=m: e.tensor_scalar(out=af[0:m, :], in0=ex[0:m, :], scalar1=sm[0:m, :], scalar2=None, op0=ALU.mult), r=['ex', 'sm'], w=['af'])
            r0 = t0 + ti * 128
            p.dma('sp', aff_d[r0:r0 + m, :], af[0:m, :], r=['af'], slot='affo')
    p.finish()
    return nc


def fm(a):
    ntok, nf = a.shape
    return np.ascontiguousarray(a.reshape(ntok, nf // 128, 128).transpose(2, 1, 0))


def tm(a):
    return np.ascontiguousarray(a.transpose(2, 1, 0).reshape(a.shape[2], -1))


def p3_inputs(inp, l, k, mix_lat, mix_ctx, xl, xc, mod_l):
    m = {}
    ls = slice(k * 1024, (k + 1) * 1024); cs = slice(k * 64, (k + 1) * 64)
    m["mixin"] = fm(np.concatenate([mix_lat[ls], mix_ctx[cs]], 0))
    m["xT"] = fm(np.concatenate([xl[ls], xc[cs]], 0))
    m["wglu"] = inp['s5_w_glu'][l]; m["bglu"] = colmajor(inp['s5_b_glu'][l])
    m["wout"] = inp['w_out'][l]; m["ssdn"] = colmajor(inp['ssd_norm'][l])
    b = k // 4
    g1 = mod_l[:, 2 * 2048:3 * 2048]; sh2 = mod_l[:, 3 * 2048:4 * 2048]; sc2 = mod_l[:, 4 * 2048:5 * 2048]
    mc = np.zeros((128, 2, 3, 16), np.float32)
    for vi, v in enumerate((b, 2)):
        mc[:, vi, 0] = colmajor(g1[v]); mc[:, vi, 1] = colmajor(sc2[v]); mc[:, vi, 2] = colmajor(sh2[v])
    m["mcol"] = mc
    m["ng"] = np.ascontiguousarray(np.stack([colmajor(inp['norm_g'][l, 1]), colmajor(inp['norm_g'][l, 2])], 1))
    m["wr"] = np.ascontiguousarray(inp['moe_router'][l].reshape(16, 128, 16).transpose(1, 0, 2))
    return m

NEXP = 16; DFF = 1536
HW = D + NEXP
NTOKA = B * NL + B * LC
CAPL = 2 * NL // NEXP
CAPC = 2 * LC // NEXP
NROW = 2 * CAPL + 2 * CAPC
BIGI = 1.0e6
SETS = [(0, NL, CAPL, 32), (NL, NL, CAPL, 32), (2 * NL, LC, CAPC, 2), (2 * NL + LC, LC, CAPC, 2)]
ROWOFF = [0, CAPL, 2 * CAPL, 2 * CAPL + CAPC]
NITER = 30


def routing(p, A, capt, sut, ones, npair, posi, tag):
    nc = p.nc
    NJ = 32
    mid = p.sb("mid", [128, npair]); lo = p.sb("lo", [128, npair]); cmp = p.sb("cmp", [128, npair, NJ]); cnt = p.sb("cnt", [128, npair])
    ge = p.sb("ge", [128, npair]); tmp = p.sb("tmp", [128, npair]); incl = p.sb("incl", [128, npair, NJ]); onej = p.sb("onej", [128, NJ])
    posf = p.sb("posf", [128, npair, NJ]); offs = p.sb("offs", [128, npair])
    ptot = p.ps("ptot", [128, 512])
    bc = lambda t: cap_(t, [[1, npair], [0, NJ]])
    p.op('dve', lambda e: e.memset(mid[:], 0.5), w=['mid'])
    p.op('dve', lambda e: e.memset(lo[:], 0.0), w=['lo'])
    p.op('dve', lambda e: e.memset(onej[:], 1.0), w=['onej'])
    w = 0.5
    for it in range(NITER):
        p.op('dve', lambda e: e.tensor_tensor(out=cmp[:], in0=A[:], in1=bc(mid), op=ALU.is_ge), r=[tag, 'mid'], w=['cmp'])
        p.op('dve', lambda e: e.tensor_reduce(out=cnt[:], in_=cmp[:], axis=AX.X, op=ALU.add), r=['cmp'], w=['cnt'])
        p.op('pe', lambda e: e.matmul(ptot[:, 0:npair], lhsT=ones[:], rhs=cnt[:], start=True, stop=True), r=['ones', 'cnt'], w=['ptot'])
        p.op('dve', lambda e: e.tensor_tensor(out=ge[:], in0=ptot[:, 0:npair], in1=capt[:], op=ALU.is_ge), r=['capt'], w=['ptot', 'ge'])
        p.op('dve', lambda e: e.tensor_tensor(out=tmp[:], in0=mid[:], in1=ge[:], op=ALU.mult), r=['mid', 'ge'], w=['tmp'])
        p.op('dve', lambda e: e.tensor_tensor(out=lo[:], in0=lo[:], in1=tmp[:], op=ALU.max), r=['tmp'], w=['lo'])
        w = w * 0.5
        p.op('dve', lambda e, w=w: e.scalar_tensor_tensor(out=mid[:], in0=ge[:], scalar=2.0 * w, in1=mid[:], op0=ALU.mult, op1=ALU.add), r=['ge'], w=['mid'])
        p.op('dve', lambda e, w=w: e.tensor_scalar(out=mid[:], in0=mid[:], scalar1=-w, scalar2=None, op0=ALU.add), w=['mid'])
    p.op('dve', lambda e: e.tensor_tensor(out=cmp[:], in0=A[:], in1=bc(lo), op=ALU.is_ge), r=[tag, 'lo'], w=['cmp'])
    for pr in range(npair):
        p.op('dve', lambda e, pr=pr: e.tensor_tensor_scan(out=incl[:, pr, :], data0=onej[:], data1=cmp[:, pr, :], initial=0.0, op0=ALU.mult, op1=ALU.add),
             r=['cmp', 'onej'], w=['incl'])
    p.op('dve', lambda e: e.tensor_copy(out=cnt[:], in_=incl[:, :, NJ - 1]), r=['incl'], w=['cnt'])
    p.op('pe', lambda e: e.matmul(ptot[:, 0:npair], lhsT=sut[:], rhs=cnt[:], start=True, stop=True), r=['sut', 'cnt'], w=['ptot'])
    p.op('dve', lambda e: e.tensor_scalar(out=offs[:], in0=ptot[:, 0:npair], scalar1=-1.0 - BIGI, scalar2=None, op0=ALU.add), w=['ptot', 'offs'])
    p.op('dve', lambda e: e.tensor_tensor(out=posf[:], in0=incl[:], in1=bc(offs), op=ALU.add), r=['incl', 'offs'], w=['posf'])
    p.op('dve', lambda e: e.tensor_tensor(out=posf[:], in0=posf[:], in1=cmp[:], op=ALU.mult), r=['cmp'], w=['posf'])
    p.op('dve', lambda e: e.tensor_scalar(out=posf[:], in0=posf[:], scalar1=BIGI, scalar2=None, op0=ALU.add), w=['posf'])
    p.op('dve', lambda e: e.tensor_copy(out=posi[:], in_=posf[:]), r=['posf'], w=['posi'])


def cap_(t, dims):
    full = t[:]
    return bass.AP(full.tensor, full.offset, [[full.ap[0][0], 128]] + [list(d) for d in dims])


def build_P4():
    p = Prog(); nc = p.nc
    h2a = p.dram("h2a", [NTOKA, HW])
    affr_d = p.dram("affr", [128, 8, 32]); capt_d = p.dram("capt", [128, 8]); sut_d = p.dram("sut", [128, 128]); ident_d = p.dram("ident", [128, 128])
    wg_d = p.dram("wg", [2, D, DFF]); wu_d = p.dram("wu", [2, D, DFF]); wd_d = p.dram("wd", [2, DFF, D])
    yexp_d = p.dram("yexp", [2, NROW, D], kind="ExternalOutput")
    pos_d = p.dram("pos", [128, 8, 32], I32, kind="ExternalOutput")
    xs_d = [[p.dram("xs_%d_%d" % (e, s), [SETS[s][2], HW], kind="Internal") for s in range(4)] for e in range(2)]

    ident = p.sb("ident", [128, 128]); ones = p.sb("ones", [128, 128]); sut = p.sb("sut", [128, 128])
    A = p.sb("A", [128, 8, 32]); capt = p.sb("capt", [128, 8]); posi = p.sb("posi", [128, 8, 32], I32)
    p.dma('sp', ident[:], ident_d, w=['ident']); p.dma('sp', sut[:], sut_d, w=['sut'])
    p.dma('sp', A[:], affr_d, w=['A']); p.dma('sp', capt[:], capt_d, w=['capt'])
    p.op('dve', lambda e: e.memset(ones[:], 1.0), w=['ones'])
    p.push()
    routing(p, A, capt, sut, ones, 8, posi, 'A')
    p.pop()
    p.dma('sp', pos_d, posi[:], r=['posi'], slot='poso')

    p.push()
    rowb = [p.sb("rowb%d" % i, [128, HW]) for i in range(3)]
    ixb = [p.sb("ixb%d" % i, [128, 1], I32) for i in range(6)]
    nr = 0; nix = 0
    breg = {c: nc.gpsimd.to_reg(c - 1) for c in (CAPL, CAPC)}
    for s, (base, ntok, cap, nj) in enumerate(SETS):
        src = h2a[base:base + ntok, :].rearrange("(p j) w -> p j w", j=nj)
        for j in range(nj):
            bi = nr % 3; nr += 1
            p.dma('sp' if bi % 2 == 0 else 'act', rowb[bi][:], src[:, j, :], w=['rowb%d' % bi], slot='rowb%d' % bi)
            for e in range(2):
                pair = s * 2 + e
                ii = nix % 6; nix += 1
                p.op('dve', lambda e_, ii=ii, pair=pair, j=j: e_.tensor_copy(out=ixb[ii][:, :], in_=posi[:, pair, j:j + 1]), r=['posi'], w=['ixb%d' % ii])
                p.idma(lambda g, e=e, s=s, bi=bi, ii=ii, cap=cap: g.indirect_dma_start(
                    out=xs_d[e][s], out_offset=bass.IndirectOffsetOnAxis(ap=ixb[ii][:, :], axis=0), in_=rowb[bi][:, :], in_offset=None,
                    bounds_check=breg[cap], oob_is_err=False), r=['rowb%d' % bi, 'ixb%d' % ii], w=[('xs', e, s, j)], slot='sc%d' % ii)

    p.pop()
    xsT = p.sb("xsT", [128, 16, NROW], BF16); hidT = p.sb("hidT", [128, 12, NROW], BF16)
    gcol = p.sb("gcol", [128, 9])
    xin = [p.sb("xin%d" % i, [128, HW]) for i in range(2)]
    wgb = [p.sb("wgb%d" % i, [128, 16, 512], BF16) for i in range(2)]; wub = [p.sb("wub%d" % i, [128, 16, 512], BF16) for i in range(2)]
    wdb = [p.sb("wdb%d" % i, [128, 12, 512], BF16) for i in range(2)]
    actb = [p.sb("actb%d" % i, [128, 512]) for i in range(2)]
    yt = [p.sb("yt%d" % i, [128, 512]) for i in range(2)]
    ptr = [p.ps("ptr%d" % i, [128, 512]) for i in range(2)]
    phg = [p.ps("phg%d" % i, [128, 512]) for i in range(2)]; phu = [p.ps("phu%d" % i, [128, 512]) for i in range(2)]
    pdn = [p.ps("pdn%d" % i, [128, 512]) for i in range(2)]
    tiles = [[(0, i * 128, 128, 0)] for i in range(4)] + [[(1, i * 128, 128, 0)] for i in range(4)] + [[(2, 0, 32, 0), (3, 0, 32, 32)]]
    nblk = [(0, 512), (512, 512), (1024, 64)]
    nx = 0; ntr = 0; nw = 0; nh = 0; nd = 0; nwd = 0
    for e in range(2):
        for ti, parts in enumerate(tiles):
            xi = nx % 2; nx += 1; xb = xin[xi]; xn = 'xin%d' % xi
            rows = sum(pp[2] for pp in parts)
            for (s, r0, nrw, po) in parts:
                p.dma('sp', xb[po:po + nrw, :], xs_d[e][s][r0:r0 + nrw, :], r=[('xs', e, s, j_) for j_ in range(SETS[s][3])], w=[xn], slot=xn + ('a' if po == 0 else 'b'))
            p.op('dve', lambda e_, xb=xb, ti=ti, rows=rows, e=e: e_.tensor_copy(out=gcol[0:rows, ti:ti + 1], in_=xb[0:rows, D + e:D + e + 1]), r=[xn], w=[('gcol', ti)])
            c0 = ROWOFF[parts[0][0]] + parts[0][1]
            for q4 in range(4):
                pi = ntr % 2; ntr += 1; pt = ptr[pi]; pn = 'ptr%d' % pi
                for j in range(4):
                    ck = q4 * 4 + j
                    p.op('pe', lambda e_, pt=pt, xb=xb, j=j, ck=ck, rows=rows: e_.transpose(out=pt[:, j * 128:j * 128 + rows], in_=xb[0:rows, ck * 128:(ck + 1) * 128],
                                                                                          identity=ident[0:rows, 0:rows]), r=[xn, 'ident'], w=[pn])
                p.op('act', lambda e_, pt=pt, q4=q4, c0=c0, rows=rows: e_.activation(
                    out=xsT[:, q4 * 4:(q4 + 1) * 4, c0:c0 + rows], in_=pt[:].rearrange("p (j n) -> p j n", n=128)[:, :, 0:rows], func=AF.Copy),
                    w=[pn, ('xsT', ti)])
        for fb in range(DFF // 512):
            wi = nw % 2; nw += 1
            p.dma('pool', wgb[wi][:], wg_d[e, :, fb * 512:(fb + 1) * 512].rearrange("(k p) n -> p k n", p=128), w=['wgb%d' % wi], slot='wgb%d' % wi)
            p.dma('pool', wub[wi][:], wu_d[e, :, fb * 512:(fb + 1) * 512].rearrange("(k p) n -> p k n", p=128), w=['wub%d' % wi], slot='wub%d' % wi)
            for m in range(4):
                fch = fb * 4 + m
                for (n0, n) in nblk:
                    hi = nh % 2; nh += 1
                    pg = phg[hi]; pu = phu[hi]
                    for k in range(16):
                        p.op('pe', lambda e_, pg=pg, wi=wi, k=k, m=m, n0=n0, n=n: e_.matmul(pg[:, 0:n], lhsT=wgb[wi][:, k, m * 128:(m + 1) * 128], rhs=xsT[:, k, n0:n0 + n],
                                                                                           start=(k == 0), stop=(k == 15)),
                             r=['wgb%d' % wi] + [('xsT', t_) for t_ in range(9)], w=['phg%d' % hi])
                    for k in range(16):
                        p.op('pe', lambda e_, pu=pu, wi=wi, k=k, m=m, n0=n0, n=n: e_.matmul(pu[:, 0:n], lhsT=wub[wi][:, k, m * 128:(m + 1) * 128], rhs=xsT[:, k, n0:n0 + n],
                                                                                           start=(k == 0), stop=(k == 15)),
                             r=['wub%d' % wi] + [('xsT', t_) for t_ in range(9)], w=['phu%d' % hi])
                    ab = actb[hi]
                    p.op('act', lambda e_, pg=pg, ab=ab, n=n: e_.activation(out=ab[:, 0:n], in_=pg[:, 0:n], func=AF.Silu), w=['phg%d' % hi, 'actb%d' % hi])
                    p.op('dve', lambda e_, pu=pu, ab=ab, fch=fch, n0=n0, n=n: e_.tensor_tensor(out=hidT[:, fch, n0:n0 + n], in0=pu[:, 0:n], in1=ab[:, 0:n], op=ALU.mult),
                         r=['actb%d' % hi], w=['phu%d' % hi, ('hidT', fch)])
        for db in range(D // 512):
            wi = nwd % 2; nwd += 1
            p.dma('pool', wdb[wi][:], wd_d[e, :, db * 512:(db + 1) * 512].rearrange("(k p) n -> p k n", p=128), w=['wdb%d' % wi], slot='wdb%d' % wi)
            for ti, parts in enumerate(tiles):
                rows = sum(pp[2] for pp in parts)
                c0 = ROWOFF[parts[0][0]] + parts[0][1]
                di = nd % 2; nd += 1; pd = pdn[di]
                for f in range(12):
                    p.op('pe', lambda e_, pd=pd, f=f, c0=c0, rows=rows, wi=wi: e_.matmul(pd[0:rows, :], lhsT=hidT[:, f, c0:c0 + rows], rhs=wdb[wi][:, f, :],
                                                                                        start=(f == 0), stop=(f == 11)),
                         r=['wdb%d' % wi] + [('hidT', f_) for f_ in range(12)], w=['pdn%d' % di])
                p.op('act', lambda e_, pd=pd, di=di, rows=rows, ti=ti: e_.activation(out=yt[di][0:rows, :], in_=pd[0:rows, :], func=AF.Copy, scale=gcol[0:rows, ti:ti + 1]),
                     r=[('gcol', ti)], w=['pdn%d' % di, 'yt%d' % di])
                p.dma('sp', yexp_d[e, c0:c0 + rows, db * 512:(db + 1) * 512], yt[di][0:rows, :], r=['yt%d' % di], slot='yo%d' % di)
    p.finish()
    return nc


GE = [0]


def p4_consts():
    k = np.arange(128)
    return {"ident": np.eye(128, dtype=np.float32), "sut": (k[:, None] < k[None, :]).astype(np.float32),
            "capt": np.ascontiguousarray(np.broadcast_to(np.array([CAPL] * 4 + [CAPC] * 4, np.float32)[None], (128, 8)))}


def p4_inputs(inp, l, k, h2_all, aff_all):
    m = {}
    e0 = 2 * k
    affk = np.concatenate([aff_all[:, e0:e0 + 2], np.zeros((NTOKA, NEXP - 2), np.float32)], 1)
    m["h2a"] = np.ascontiguousarray(np.concatenate([h2_all, affk], 1))
    A = np.full((128, 8, 32), -1.0, np.float32)
    for s, (base, ntok, cap, nj) in enumerate(SETS):
        for e in range(2):
            A[:, s * 2 + e, 0:nj] = aff_all[base:base + ntok, e0 + e].reshape(128, nj)
    m["affr"] = A
    m["wg"] = inp['moe_w_gate'][l, e0:e0 + 2]; m["wu"] = inp['moe_w_up'][l, e0:e0 + 2]; m["wd"] = inp['moe_w_down'][l, e0:e0 + 2]
    m.update(p4_consts())
    return m

NT5 = 9


def build_P5():
    p = Prog(); nc = p.nc
    x1_d = p.dram("x1", [1088, D])
    ylat_d = [p.dram("ylat%d" % e, [CAPL, D]) for e in range(NEXP)]
    yctx_d = [p.dram("yctx%d" % e, [CAPC, D]) for e in range(NEXP)]
    pos_d = p.dram("pos5", [128, NT5, NEXP], I32)
    gm_d = p.dram("gm", [128, 2, 2, D])
    x2_d = p.dram("x2", [1088, D], kind="ExternalOutput")
    pos = p.sb("pos", [128, NT5, NEXP], I32)
    gm = p.sb("gm", [128, 2, 2, D]); Gm = p.sb("Gm", [128, 2, D])
    p.dma('sp', pos[:], pos_d, w=['pos']); p.dma('sp', gm[:], gm_d, w=['gm'])
    for v in range(2):
        p.op('dve', lambda e, v=v: e.tensor_tensor(out=Gm[:, v, :], in0=gm[:, v, 0, :], in1=gm[:, v, 1, :], op=ALU.mult), r=['gm'], w=['Gm'])
    gb = [p.sb("gb%d" % i, [128, D]) for i in range(4)]
    ixb = [p.sb("ixb%d" % i, [128, 1], I32) for i in range(4)]
    acc = [p.sb("acc%d" % i, [128, D]) for i in range(2)]
    x1 = [p.sb("x1%d" % i, [128, D]) for i in range(2)]
    junk = p.sb("junk", [128, D], BF16)
    ss = p.sb("ss", [128, NT5]); rs = p.sb("rs", [128, NT5])
    breg = {c: nc.gpsimd.to_reg(c - 1) for c in (CAPL, CAPC)}
    ng = 0
    for t in range(NT5):
        rows = 128 if t < 8 else 64
        v = 0 if t < 8 else 1
        capn = CAPL if t < 8 else CAPC
        r0 = t * 128
        ai = t % 2; ac = acc[ai]; an = 'acc%d' % ai
        xi = t % 2
        p.dma('sp', x1[xi][0:rows, :], x1_d[r0:r0 + rows, :], w=['x1%d' % xi], slot='x1%d' % xi)
        for e in range(NEXP):
            gi = ng % 4; ng += 1
            src = ylat_d[e] if t < 8 else yctx_d[e]
            p.op('pool', lambda e_, gi=gi: e_.memset(gb[gi][:], 0.0), w=['gb%d' % gi])
            p.op('dve', lambda e_, gi=gi, t=t, e=e: e_.tensor_copy(out=ixb[gi][:, :], in_=pos[:, t, e:e + 1]), r=['pos'], w=['ixb%d' % gi])
            p.idma(lambda g, gi=gi, src=src, rows=rows, capn=capn: g.indirect_dma_start(
                out=gb[gi][0:rows, :], out_offset=None, in_=src, in_offset=bass.IndirectOffsetOnAxis(ap=ixb[gi][0:rows, :], axis=0),
                bounds_check=breg[capn], oob_is_err=False), r=['ixb%d' % gi], w=['gb%d' % gi], slot='g%d' % gi)
            if e == 0:
                p.op('dve', lambda e_, gi=gi, ac=ac: e_.tensor_copy(out=ac[:], in_=gb[gi][:]), r=['gb%d' % gi], w=[an])
            else:
                p.op('dve', lambda e_, gi=gi, ac=ac: e_.tensor_tensor(out=ac[:], in0=ac[:], in1=gb[gi][:], op=ALU.add), r=['gb%d' % gi], w=[an])
        p.op('act', lambda e_, ac=ac, t=t: e_.activation(out=junk[:], in_=ac[:], func=AF.Square, accum_out=ss[:, t:t + 1]), r=[an], w=['junk', ('ss', t)])
        p.op('dve', lambda e_, t=t: e_.tensor_scalar(out=rs[:, t:t + 1], in0=ss[:, t:t + 1], scalar1=1.0 / D, scalar2=EPS, op0=ALU.mult, op1=ALU.add), r=[('ss', t)], w=[('rs', t)])
        p.op('act', lambda e_, t=t: e_.activation(out=rs[:, t:t + 1], in_=rs[:, t:t + 1], func=AF.Sqrt), w=[('rs', t)])
        p.op('dve', lambda e_, t=t: e_.reciprocal(out=rs[:, t:t + 1], in_=rs[:, t:t + 1]), w=[('rs', t)])
        p.op('dve', lambda e_, ac=ac, t=t, v=v: e_.scalar_tensor_tensor(out=ac[:], in0=ac[:], scalar=rs[:, t:t + 1], in1=Gm[:, v, :], op0=ALU.mult, op1=ALU.mult),
             r=[('rs', t), 'Gm'], w=[an])
        p.op('pool', lambda e_, ac=ac, xi=xi: e_.tensor_tensor(out=ac[:], in0=ac[:], in1=x1[xi][:], op=ALU.add), r=['x1%d' % xi], w=[an])
        p.dma('sp', x2_d[r0:r0 + rows, :], ac[0:rows, :], r=[an], slot='x2o%d' % ai)
    p.finish()
    return nc


_PROGS = {}


def _prog(name, fn):
    if name not in _PROGS:
        _PROGS[name] = fn()
    return _PROGS[name]


def _run(nc, maps):
    res = run_bass_kernel_spmd(nc, maps, core_ids=list(range(NCORES)))
    return res.results


def to_scan(x):
    b, n = x.shape[:2]
    return np.ascontiguousarray(x.reshape(b, n // 64, 64, -1).swapaxes(1, 2).reshape(x.shape))


def from_scan(x):
    b, n = x.shape[:2]
    return np.ascontiguousarray(x.reshape(b, 64, n // 64, -1).swapaxes(1, 2).reshape(x.shape))


def run_A_cached(inp):
    nc = _prog('A', build_A)
    NCOL = 1536
    cv = np.stack([inp['c'][0], inp['c'][1], inp['c_ctx']], 0).astype(np.float32)
    cT = np.ascontiguousarray(cv.reshape(3, 16, 128).transpose(2, 0, 1))
    maps = []
    for k in range(NCORES):
        maps.append({"cT": cT, "adaw": np.ascontiguousarray(inp['ada_w'][:, :, k * NCOL:(k + 1) * NCOL]),
                     "adab": np.ascontiguousarray(inp['ada_b'][:, k * NCOL:(k + 1) * NCOL]).reshape(1, -1), "ones": np.ones((1, 4), np.float32)})
    res = _run(nc, maps)
    mod = np.zeros((DEPTH, 3, 12288), np.float32)
    for k in range(NCORES):
        mod[:, :, k * NCOL:(k + 1) * NCOL] = res[k]["mod"].reshape(3, DEPTH, NCOL).transpose(1, 0, 2)
    return mod


def run_layer(inp, l, xl, xc, mod_l, dbg=None):
    col = l % 2 == 1
    xl_s = to_scan(xl) if col else xl
    res = _run(_prog('P2', build_P2), [p2_inputs(inp, l, k, xl_s, xc, mod_l) for k in range(NCORES)])
    mix = np.zeros((B, T, 2048), np.float32)
    for k in range(NCORES):
        mix[:, :, k * 128:(k + 1) * 128] = res[k]["ys5"].reshape(8, L5, 16, B, NCH).transpose(3, 4, 1, 0, 2).reshape(B, T, 128)
        mix[:, :, 1024 + k * 128:1024 + (k + 1) * 128] = res[k]["yssd"].reshape(128, B, T).transpose(1, 2, 0)
    mlat = mix[:, LC:]
    if col:
        mlat = from_scan(mlat)
    mix_lat = np.ascontiguousarray(mlat).reshape(-1, 2048); mix_ctx = np.ascontiguousarray(mix[:, :LC]).reshape(-1, 2048)
    if dbg: dbg('mix', l, mix_lat=mix_lat, mix_ctx=mix_ctx)
    xlf = xl.reshape(-1, D); xcf = xc.reshape(-1, D)
    res = _run(_prog('P3', build_P3), [p3_inputs(inp, l, k, mix_lat, mix_ctx, xlf, xcf, mod_l) for k in range(NCORES)])
    x1_lat = np.zeros((B * NL, D), np.float32); x1_ctx = np.zeros((B * LC, D), np.float32)
    h2_all = np.zeros((NTOKA, D), np.float32); aff_all = np.zeros((NTOKA, NEXP), np.float32)
    for k in range(NCORES):
        x1 = tm(res[k]["x1T"]); h2 = tm(res[k]["h2T"]); af = res[k]["aff"]
        ls = slice(k * 1024, (k + 1) * 1024); cs = slice(k * 64, (k + 1) * 64); cs2 = slice(B * NL + k * 64, B * NL + (k + 1) * 64)
        x1_lat[ls] = x1[:1024]; x1_ctx[cs] = x1[1024:]
        h2_all[ls] = h2[:1024]; h2_all[cs2] = h2[1024:]
        aff_all[ls] = af[:1024]; aff_all[cs2] = af[1024:]
    if dbg: dbg('p3', l, x1_lat=x1_lat, x1_ctx=x1_ctx, h2_all=h2_all, aff_all=aff_all)
    res = _run(_prog('P4', build_P4), [p4_inputs(inp, l, k, h2_all, aff_all) for k in range(NCORES)])
    yexp = [res[k]["yexp"] for k in range(NCORES)]
    posfull = [np.zeros((SETS[s][1], NEXP), np.int32) for s in range(4)]
    for k in range(NCORES):
        pk = res[k]["pos"]
        for s in range(4):
            nj = SETS[s][3]
            for el in range(2):
                posfull[s][:, 2 * k + el] = pk[:, s * 2 + el, 0:nj].reshape(-1)
    g2 = mod_l[:, 5 * 2048:6 * 2048]
    ng3 = inp['norm_g'][l, 3]
    maps = []
    for k in range(NCORES):
        b = k // 4; kk = k % 4
        m = {}
        m["x1"] = np.ascontiguousarray(np.concatenate([x1_lat[k * 1024:(k + 1) * 1024], x1_ctx[k * 64:(k + 1) * 64]], 0))
        for e in range(NEXP):
            m["ylat%d" % e] = np.ascontiguousarray(yexp[e // 2][e % 2, ROWOFF[b]:ROWOFF[b] + CAPL])
            m["yctx%d" % e] = np.ascontiguousarray(yexp[e // 2][e % 2, ROWOFF[2 + b]:ROWOFF[2 + b] + CAPC])
        p5 = np.full((128, NT5, NEXP), int(BIGI), np.int32)
        p5[:, 0:8, :] = posfull[b][kk * 1024:(kk + 1) * 1024].reshape(8, 128, NEXP).transpose(1, 0, 2)
        p5[0:64, 8, :] = posfull[2 + b][kk * 64:(kk + 1) * 64]
        m["pos5"] = p5
        gm = np.zeros((128, 2, 2, D), np.float32)
        gm[:, 0, 0] = g2[b][None]; gm[:, 1, 0] = g2[2][None]; gm[:, :, 1] = ng3[None, None]
        m["gm"] = gm
        maps.append(m)
    res = _run(_prog('P5', build_P5), maps)
    xl2 = np.zeros((B * NL, D), np.float32); xc2 = np.zeros((B * LC, D), np.float32)
    for k in range(NCORES):
        x2 = res[k]["x2"]
        xl2[k * 1024:(k + 1) * 1024] = x2[:1024]; xc2[k * 64:(k + 1) * 64] = x2[1024:]
    return xl2.reshape(B, NL, D), xc2.reshape(B, LC, D)


def kernel(x, c, ctx, c_ctx, ada_w, ada_b, norm_g, w_in, w_out, s5_lam_re, s5_lam_im, s5_log_step,
           s5_b_re, s5_b_im, s5_c_re, s5_c_im, s5_d, s5_w_glu, s5_b_glu, ssd_conv_w, ssd_conv_b,
           ssd_dt_bias, ssd_a_log, ssd_d, ssd_norm, moe_router, moe_w_gate, moe_w_up, moe_w_down, _dbg=None):
    inp = dict(x=x, c=c, ctx=ctx, c_ctx=c_ctx, ada_w=ada_w, ada_b=ada_b, norm_g=norm_g, w_in=w_in, w_out=w_out,
               s5_lam_re=s5_lam_re, s5_lam_im=s5_lam_im, s5_log_step=s5_log_step, s5_b_re=s5_b_re, s5_b_im=s5_b_im,
               s5_c_re=s5_c_re, s5_c_im=s5_c_im, s5_d=s5_d, s5_w_glu=s5_w_glu, s5_b_glu=s5_b_glu, ssd_conv_w=ssd_conv_w,
               ssd_conv_b=ssd_conv_b, ssd_dt_bias=ssd_dt_bias, ssd_a_log=ssd_a_log, ssd_d=ssd_d, ssd_norm=ssd_norm,
               moe_router=moe_router, moe_w_gate=moe_w_gate, moe_w_up=moe_w_up, moe_w_down=moe_w_down)
    inp = {k: np.asarray(v, dtype=np.float32) for k, v in inp.items()}
    mod = run_A_cached(inp)
    xl = np.ascontiguousarray(inp['x']); xc = np.ascontiguousarray(inp['ctx'])
    for l in range(DEPTH):
        xl, xc = run_layer(inp, l, xl, xc, mod[l], _dbg)
        if _dbg: _dbg('layer', l, xl=xl, xc=xc)
    return np.ascontiguousarray(xl.astype(np.float32))
```
